# Optimizing a Trainium2 kernel written in Bass

```python
import jax, jax.numpy as jnp
from jax import lax
import numpy as np

D_MODEL = 1024
BATCH = 16
SEQ = 4096
DEPTH = 1

N_MEM = 256
HEAD_DIM = 64
ATT_HEADS = 4
ATT_WIDTH = ATT_HEADS * HEAD_DIM
CONV_WIDTH = 384
CONV_KERNEL = 31
CONV_PAD = CONV_KERNEL // 2
RWKV_HEADS = 6
RWKV_WIDTH = RWKV_HEADS * HEAD_DIM
MIX_WIDTH = ATT_WIDTH + CONV_WIDTH + RWKV_WIDTH
DECAY_RANK = 64
ICLR_RANK = 64
GATE_RANK = 128
RWKV_PROJ = 3 * RWKV_WIDTH + 2 * DECAY_RANK + 2 * ICLR_RANK + GATE_RANK
IN_PROJ = ATT_WIDTH + 2 * CONV_WIDTH + RWKV_PROJ
N_EXPERTS = 16
EXPERT_FF = 1024
CAPACITY_FACTOR = 2
NORM_EPS = 1e-6
LN_EPS = 1e-5
GN_EPS = 64e-5
L2_EPS = 1e-12
DECAY_SCALE = 0.606531

kernel_name = "hybrid_conformer_rwkv7_memxattn_ec_moe"


def rms_norm(x, g):
    xf = x.astype(jnp.float32)
    y = xf * lax.rsqrt(jnp.mean(xf * xf, axis=-1, keepdims=True) + NORM_EPS)
    return (y * g.astype(jnp.float32)).astype(x.dtype)


def memory_cross_attention(q, mem_n, w_mk, w_mv):
    B, S, _ = q.shape
    qh = q.reshape(B, S, ATT_HEADS, HEAD_DIM)
    kh = (mem_n @ w_mk).reshape(B, -1, ATT_HEADS, HEAD_DIM)
    vh = (mem_n @ w_mv).reshape(B, -1, ATT_HEADS, HEAD_DIM)
    s = jnp.einsum('bqhd,bmhd->bhqm', qh, kh).astype(jnp.float32) * (HEAD_DIM ** -0.5)
    pr = jax.nn.softmax(s, axis=-1).astype(vh.dtype)
    o = jnp.einsum('bhqm,bmhd->bqhd', pr, vh)
    return o.reshape(B, S, ATT_WIDTH)


def conformer_conv(pc, conv_w, conv_b, ln_g, ln_b):
    val, gate = jnp.split(pc, 2, axis=-1)
    u = val * jax.nn.sigmoid(gate)
    u = lax.conv_general_dilated(
        u, conv_w[:, None, :].astype(u.dtype), window_strides=(1,),
        padding=[(CONV_PAD, CONV_PAD)], dimension_numbers=('NWC', 'WIO', 'NWC'),
        feature_group_count=CONV_WIDTH) + conv_b.astype(u.dtype)
    uf = u.astype(jnp.float32)
    mu = jnp.mean(uf, axis=-1, keepdims=True)
    var = jnp.mean(jnp.square(uf - mu), axis=-1, keepdims=True)
    uf = (uf - mu) * lax.rsqrt(var + LN_EPS) * ln_g.astype(jnp.float32) + ln_b.astype(jnp.float32)
    return jax.nn.silu(uf).astype(pc.dtype)


def token_shift_centred(p, mu):
    prev = jnp.pad(p, ((0, 0), (1, 0), (0, 0)))[:, :-1]
    nxt = jnp.pad(p, ((0, 0), (0, 1), (0, 0)))[:, 1:]
    return p + mu[0] * (prev - p) + mu[1] * (nxt - p)


def wkv7_scan(r, w, k, v, kk, a, reverse):
    def step(state, inp):
        r_t, w_t, k_t, v_t, kk_t, a_t = inp
        sa = jnp.einsum('bhij,bhj->bhi', state, -kk_t)
        state = (state * w_t[..., None, :]
                 + sa[..., :, None] * (kk_t * a_t)[..., None, :]
                 + v_t[..., :, None] * k_t[..., None, :])
        y_t = jnp.einsum('bhij,bhj->bhi', state, r_t)
        return state, y_t
    S, B, H, N = r.shape
    state0 = jnp.zeros((B, H, N, N), jnp.float32)
    _, ys = lax.scan(step, state0, (r, w, k, v, kk, a), reverse=reverse)
    return ys


def rwkv7_bidirectional(pr, shift_mu, decay_w0, decay_up, iclr_a0, iclr_up, gate_up,
                        k_k, k_a, r_k, lnx_g, lnx_b):
    f32 = jnp.float32
    B, S, _ = pr.shape
    pf = token_shift_centred(pr.astype(f32), shift_mu.astype(f32))
    c0 = 3 * RWKV_WIDTH
    cuts = [RWKV_WIDTH, 2 * RWKV_WIDTH, c0, c0 + DECAY_RANK, c0 + 2 * DECAY_RANK,
            c0 + 2 * DECAY_RANK + ICLR_RANK, c0 + 2 * DECAY_RANK + 2 * ICLR_RANK]
    r, k, v, wdf, wdb, adf, adb, gd = jnp.split(pf, cuts, axis=-1)

    def heads(t):
        return t.reshape(t.shape[:-1] + (RWKV_HEADS, HEAD_DIM))

    def tm(t):
        return jnp.moveaxis(t, 1, 0)

    g = jax.nn.sigmoid(gd) @ gate_up.astype(f32)
    kk = heads(k * k_k.astype(f32))
    kk = kk * lax.rsqrt(jnp.sum(kk * kk, axis=-1, keepdims=True) + L2_EPS)
    r_h, v_h = heads(r), heads(v)
    r_tm, v_tm, kk_tm = tm(r_h), tm(v_h), tm(kk)
    r_k32 = r_k.astype(f32)

    outs, bonuses = [], []
    for d, (wd, ad) in enumerate(((wdf, adf), (wdb, adb))):
        w = jnp.exp(-DECAY_SCALE * jax.nn.sigmoid(
            decay_w0[d].astype(f32) + jnp.tanh(wd) @ decay_up[d].astype(f32)))
        a = jax.nn.sigmoid(iclr_a0[d].astype(f32) + ad @ iclr_up[d].astype(f32))
        k_d = heads(k * (1.0 + (a - 1.0) * k_a.astype(f32)))
        ys = wkv7_scan(r_tm, tm(heads(w)), tm(k_d), v_tm, kk_tm, tm(heads(a)), reverse=(d == 1))
        outs.append(jnp.moveaxis(ys, 0, 1))
        bonuses.append(jnp.sum(r_h * k_d * r_k32, axis=-1, keepdims=True) * v_h)
    y = outs[0] + outs[1]
    mu = jnp.mean(y, axis=-1, keepdims=True)
    var = jnp.mean(jnp.square(y - mu), axis=-1, keepdims=True)
    y = ((y - mu) * lax.rsqrt(var + GN_EPS) * lnx_g.astype(f32).reshape(RWKV_HEADS, HEAD_DIM)
         + lnx_b.astype(f32).reshape(RWKV_HEADS, HEAD_DIM))
    y = (y + bonuses[0] + bonuses[1]).reshape(B, S, RWKV_WIDTH) * g
    return y.astype(pr.dtype)


def expert_choice_ffn(h, w_router, w_gate, w_up, w_down):
    B, S, D = h.shape
    cap = CAPACITY_FACTOR * S // N_EXPERTS
    aff = jax.nn.softmax((h @ w_router).astype(jnp.float32), axis=-1)
    gates, idx = lax.top_k(jnp.swapaxes(aff, 1, 2), cap)
    xe = jax.vmap(lambda hb, ib: hb[ib])(h, idx)
    hid = jax.nn.silu(jnp.einsum('becd,edf->becf', xe, w_gate)) * jnp.einsum('becd,edf->becf', xe, w_up)
    ye = jnp.einsum('becf,efd->becd', hid, w_down) * gates[..., None].astype(h.dtype)
    bidx = jnp.arange(B)[:, None, None]
    return jnp.zeros_like(h).at[bidx, idx].add(ye)


def setup_inputs(seed: int = 0) -> dict:
    key = jax.random.key(seed)
    ks = iter(jax.random.split(key, 40))
    L, D = DEPTH, D_MODEL

    def nrm(shape, scale):
        return scale * jax.random.normal(next(ks), shape, jnp.float32)

    return {
        "x": nrm((BATCH, SEQ, D), 1.0),
        "mem": nrm((BATCH, N_MEM, D), 1.0),
        "norm_mix_g": 1.0 + nrm((L, D), 0.05),
        "norm_mem_g": 1.0 + nrm((L, D), 0.05),
        "w_in": nrm((L, D, IN_PROJ), D ** -0.5),
        "w_mk": nrm((L, D, ATT_WIDTH), D ** -0.5),
        "w_mv": nrm((L, D, ATT_WIDTH), D ** -0.5),
        "conv_w": nrm((L, CONV_KERNEL, CONV_WIDTH), CONV_KERNEL ** -0.5),
        "conv_b": nrm((L, CONV_WIDTH), 0.01),
        "conv_ln_g": 1.0 + nrm((L, CONV_WIDTH), 0.05),
        "conv_ln_b": nrm((L, CONV_WIDTH), 0.01),
        "shift_mu": jax.random.uniform(next(ks), (L, 2, RWKV_PROJ), jnp.float32, 0.0, 0.5),
        "decay_w0": nrm((L, 2, RWKV_WIDTH), 0.5),
        "decay_up": nrm((L, 2, DECAY_RANK, RWKV_WIDTH), 0.1),
        "iclr_a0": nrm((L, 2, RWKV_WIDTH), 0.5),
        "iclr_up": nrm((L, 2, ICLR_RANK, RWKV_WIDTH), 0.1),
        "gate_up": nrm((L, GATE_RANK, RWKV_WIDTH), GATE_RANK ** -0.5),
        "k_k": 0.85 + nrm((L, RWKV_WIDTH), 0.05),
        "k_a": 1.0 + nrm((L, RWKV_WIDTH), 0.05),
        "r_k": nrm((L, RWKV_HEADS, HEAD_DIM), 0.1),
        "lnx_g": 1.0 + nrm((L, RWKV_WIDTH), 0.05),
        "lnx_b": nrm((L, RWKV_WIDTH), 0.01),
        "w_out": nrm((L, MIX_WIDTH, D), MIX_WIDTH ** -0.5),
        "norm_ffn_g": 1.0 + nrm((L, D), 0.05),
        "w_router": nrm((L, D, N_EXPERTS), D ** -0.5),
        "w_e_gate": nrm((L, N_EXPERTS, D, EXPERT_FF), D ** -0.5),
        "w_e_up": nrm((L, N_EXPERTS, D, EXPERT_FF), D ** -0.5),
        "w_e_down": nrm((L, N_EXPERTS, EXPERT_FF, D), EXPERT_FF ** -0.5),
        "final_norm_g": 1.0 + nrm((D,), 0.05),
    }


def reference(x, mem, norm_mix_g, norm_mem_g, w_in, w_mk, w_mv, conv_w, conv_b, conv_ln_g,
              conv_ln_b, shift_mu, decay_w0, decay_up, iclr_a0, iclr_up, gate_up, k_k, k_a,
              r_k, lnx_g, lnx_b, w_out, norm_ffn_g, w_router, w_e_gate, w_e_up, w_e_down,
              final_norm_g):
    for l in range(DEPTH):
        h = rms_norm(x, norm_mix_g[l])
        p = h @ w_in[l]
        q, pc, pr = jnp.split(p, [ATT_WIDTH, ATT_WIDTH + 2 * CONV_WIDTH], axis=-1)
        mem_n = rms_norm(mem, norm_mem_g[l])
        o_att = memory_cross_attention(q, mem_n, w_mk[l], w_mv[l])
        o_conv = conformer_conv(pc, conv_w[l], conv_b[l], conv_ln_g[l], conv_ln_b[l])
        o_rwkv = rwkv7_bidirectional(pr, shift_mu[l], decay_w0[l], decay_up[l], iclr_a0[l],
                                     iclr_up[l], gate_up[l], k_k[l], k_a[l], r_k[l],
                                     lnx_g[l], lnx_b[l])
        x = x + jnp.concatenate([o_att, o_conv, o_rwkv], axis=-1) @ w_out[l]
        x = x + expert_choice_ffn(rms_norm(x, norm_ffn_g[l]), w_router[l], w_e_gate[l],
                                  w_e_up[l], w_e_down[l])
    return rms_norm(x, final_norm_g)
```

```python
import numpy as np
from contextlib import ExitStack
import concourse.bass as bass
import concourse.mybir as mybir
from concourse.bass_utils import run_bass_kernel_spmd

F32 = mybir.dt.float32
BF16 = mybir.dt.bfloat16
U32 = mybir.dt.uint32
AF = mybir.ActivationFunctionType
OP = mybir.AluOpType

D = 1024
NMEM = 256
INP = 2560
E = 16
FF = 1024
SEM_LIMIT = 30000


class Ctr:
    def __init__(self, prog, name, step):
        self.prog, self.name, self.step = prog, name, step
        self.sem = None
        self.val = 0
        self.n = 0

    def next(self):
        if self.sem is None and self.step == 16 and self.prog.free_dma_sems and not getattr(self, "sw", False):
            self.sem, self.val = self.prog.free_dma_sems.pop()
        if self.sem is None or self.val + self.step > SEM_LIMIT:
            self.sem = self.prog.new_sem(f"s{self.prog.nsem}")
            if self.name == "pe":
                self.prog.pe_sems.add(id(self.sem))
            self.n += 1
            self.val = 0
        self.val += self.step
        return (self.sem, self.val)


class Prog:
    ENG = ("pe", "act", "dve", "pool", "sp")

    def __init__(self, nc, stack):
        self.nc, self.stack = nc, stack
        self.ops = {e: [] for e in self.ENG}
        self.ctr = {e: Ctr(self, e, 1) for e in self.ENG}
        self.dctr = {}
        self.lastw = {}
        self.reads = {}
        self.known = {e: {} for e in self.ENG}
        self.nsem = 0
        self.all_events = {}
        self.pe_sems = set()
        self.free_dma_sems = []

    def new_sem(self, name):
        self.nsem += 1
        return self.stack.enter_context(self.nc.semaphore(name))

    def _deps(self, eng, r, w, force=False):
        deps = []
        for k in r:
            ev = self.lastw.get(k)
            if ev is not None:
                deps.append(ev)
        for k in w:
            ev = self.lastw.get(k)
            if ev is not None:
                deps.append(ev)
            deps.extend(self.reads.get(k, {}).items())
        waits = []
        kn = self.known[eng]
        for sem, val in deps:
            if eng == "pe" and id(sem) in self.pe_sems and not force:
                continue
            if kn.get(sem, 0) >= val:
                continue
            kn[sem] = val
            waits.append((sem, val))
        best = {}
        for sem, val in waits:
            best[sem] = max(best.get(sem, 0), val)
        return list(best.items())

    def _commit(self, ev, r, w):
        sem, val = ev
        self.all_events[sem] = val
        for k in r:
            d = self.reads.setdefault(k, {})
            d[sem] = max(d.get(sem, 0), val)
        for k in w:
            self.lastw[k] = ev
            self.reads[k] = {}

    def op(self, eng, fn, r=(), w=(), force=False):
        waits = self._deps(eng, r, w, force)
        ev = self.ctr[eng].next()
        self.ops[eng].append((waits, fn, ev, 1))
        self._commit(ev, r, w)

    def dma(self, eng, fn, r=(), w=(), stream=None):
        waits = self._deps(eng, r, w)
        if stream is None:
            stream = "d_" + str((tuple(w) or tuple(r))[0])
        c = self.dctr.get(stream)
        if c is None:
            c = self.dctr[stream] = Ctr(self, stream, 16)
            c.sw = (eng == "pool")
        ev = c.next()
        self.ops[eng].append((waits, fn, ev, 16))
        self._commit(ev, r, w)

    def barrier(self):
        for eng in self.ENG:
            waits = []
            kn = self.known[eng]
            for sem, val in self.all_events.items():
                if kn.get(sem, 0) >= val:
                    continue
                kn[sem] = val
                waits.append((sem, val))
            if waits:
                self.ops[eng].append((waits, None, None, 0))
        self.lastw.clear()
        self.reads.clear()
        for c in self.dctr.values():
            if c.sem is not None and not getattr(c, "sw", False):
                self.free_dma_sems.append((c.sem, c.val))
        self.dctr.clear()

    def emit(self):
        nc = self.nc
        with nc.Block() as block:
            def mk(name):
                def body(engobj):
                    for waits, fn, ev, step in self.ops[name]:
                        for sem, val in waits:
                            engobj.wait_ge(sem, val)
                        if fn is not None:
                            inst = fn(engobj)
                            inst.then_inc(ev[0], step)
                return body
            block.tensor(mk("pe"))
            block.scalar(mk("act"))
            block.vector(mk("dve"))
            block.gpsimd(mk("pool"))
            block.sync(mk("sp"))


class Builder:
    def __init__(self, S, NB, debug=False):
        self.S, self.NB, self.T = S, NB, S * NB
        self.debug = debug
        self.nc = bass.Bass("TRN2", target_bir_lowering=False)
        self.stack = ExitStack()
        self.P = Prog(self.nc, self.stack)
        self.uid = 0
        self.dbg_outs = []

    def dram_in(self, name, shape, dt=F32):
        return self.nc.dram_tensor(name, list(shape), dt, kind="ExternalInput").ap()

    def dram_out(self, name, shape, dt=F32):
        return self.nc.dram_tensor(name, list(shape), dt, kind="ExternalOutput").ap()

    def scratch(self, name, shape, dt=F32, dbg=False):
        if self.debug and dbg:
            self.dbg_outs.append(name)
            return self.nc.dram_tensor(name, list(shape), dt, kind="ExternalOutput").ap()
        return self.nc.dram_tensor(name, list(shape), dt, kind="Internal").ap()

    def sb(self, st, name, shape, dt=F32):
        return st.enter_context(self.nc.sbuf_tensor(name, list(shape), dt))

    def ps(self, st, name, shape, dt=F32):
        return st.enter_context(self.nc.psum_tensor(name, list(shape), dt))

    def mm(self, out, lhsT, rhs, start, stop, r, w, force=False):
        self.P.op("pe", lambda e: e.matmul(out, lhsT, rhs, start=start, stop=stop), r=r, w=w, force=force)

    def tr(self, out, in_, ident, r, w):
        self.P.op("pe", lambda e: e.transpose(out, in_, ident), r=r, w=w)

    def act(self, out, in_, func, r, w, bias=None, scale=None, accum_out=None):
        kw = {}
        if bias is not None:
            kw["bias"] = bias
        if scale is not None:
            kw["scale"] = scale
        if accum_out is not None:
            kw["accum_out"] = accum_out
        self.P.op("act", lambda e: e.activation(out, in_, func, **kw), r=r, w=w)

    def tt(self, eng, out, in0, in1, op, r, w):
        self.P.op(eng, lambda e: e.tensor_tensor(out, in0, in1, op), r=r, w=w)

    def ts(self, eng, out, in0, s1, op0, r, w, s2=None, op1=None):
        if op1 is None:
            self.P.op(eng, lambda e: e.tensor_scalar(out, in0, s1, None, op0), r=r, w=w)
        else:
            self.P.op(eng, lambda e: e.tensor_scalar(out, in0, s1, s2, op0, op1), r=r, w=w)

    def stt(self, out, in0, scalar, in1, op0, op1, r, w):
        self.P.op("dve", lambda e: e.scalar_tensor_tensor(out, in0, scalar, in1, op0, op1), r=r, w=w)

    def cp(self, eng, out, in_, r, w):
        if eng == "act":
            self.P.op("act", lambda e: e.copy(out, in_), r=r, w=w)
        else:
            self.P.op(eng, lambda e: e.tensor_copy(out, in_), r=r, w=w)

    def memset(self, eng, out, val, w):
        self.P.op(eng, lambda e: e.memset(out, val), r=(), w=w)

    def ld(self, out, in_, r, w, eng="sp", stream=None, nc_ok=False):
        kw = {"allow_slow_non_contiguous": True} if nc_ok else {}
        self.P.dma(eng, lambda e: e.dma_start(out=out, in_=in_, **kw), r=r, w=w, stream=stream)

    def st_dram(self, out, in_, r, w, eng="sp", stream=None):
        if stream is None:
            stream = "st_" + str(r[0])
        self.P.dma(eng, lambda e: e.dma_start(out=out, in_=in_), r=r, w=w, stream=stream)

    def declare_io(self):
        T, NB = self.T, self.NB
        I = {}
        I["x"] = self.dram_in("x", [T, D])
        I["mem"] = self.dram_in("mem", [NB * NMEM, D])
        for nm, shp in [("norm_mix_g", [D]), ("norm_mem_g", [D]), ("w_in", [D, INP]), ("w_mk", [D, 256]),
                        ("w_mv", [D, 256]), ("conv_w", [31, 384]), ("conv_b", [384]), ("conv_ln_g", [384]),
                        ("conv_ln_b", [384]), ("shift_mu", [2, 1536]), ("decay_w0", [2, 384]),
                        ("decay_up", [128, 384]), ("iclr_a0", [2, 384]), ("iclr_up", [128, 384]),
                        ("gate_up", [128, 384]), ("k_k", [384]), ("k_a", [384]), ("r_k", [384]),
                        ("lnx_g", [384]), ("lnx_b", [384]), ("w_out", [D, D]), ("norm_ffn_g", [D]),
                        ("w_router", [D, E]), ("w_e_gate", [E, D, FF]), ("w_e_up", [E, D, FF]),
                        ("w_e_down", [E, FF, D]), ("final_norm_g", [D])]:
            I[nm] = self.dram_in(nm, shp)
        self.I = I
        self.out = self.dram_out("out", [T, D])
        self.pT = self.scratch("pT", [INP, T], F32, dbg=True)
        self.mixT = self.scratch("mixT", [D, T], BF16, dbg=True)
        self.FM = [self.scratch(f"FM{d}", [4, 384, T], F32, dbg=True) for d in range(2)]
        self.TM = [self.scratch(f"TM{d}", [T, 3, 384], F32, dbg=True) for d in range(2)]
        self.TMV = self.scratch("TMV", [T, 384], F32, dbg=True)
        self.PLd = [self.scratch(f"PL{d}", [384, T // 64], F32, dbg=True) for d in range(2)]
        self.gT = self.scratch("gT", [384, T], F32, dbg=True)
        self.bonT = self.scratch("bonT", [384, T], F32, dbg=True)
        self.YT = [self.scratch(f"YT{d}", [384, T], F32, dbg=True) for d in range(2)]
        self.x1d = self.scratch("x1d", [T, D], F32, dbg=True)
        self.h2d = self.scratch("h2d", [T, D], BF16, dbg=True)

    def consts(self, st):
        B = self
        io = B.sb(st, "c_iota", [128, 128], mybir.dt.int32)
        B.P.op("pool", lambda e: e.iota(io[:, :], [[1, 128]], base=0, channel_multiplier=-1), w=["c_iota"])
        B.idf = B.sb(st, "c_idf", [128, 128], F32)
        B.idb = B.sb(st, "c_idb", [128, 128], BF16)
        B.dif = B.sb(st, "c_dif", [128, 128], F32)
        B.cp("dve", B.dif[:, :], io[:, :], r=["c_iota"], w=["c_dif"])
        B.ts("dve", B.idf[:, :], B.dif[:, :], 0.0, OP.is_equal, r=["c_dif"], w=["c_idf"])
        B.cp("dve", B.idb[:, :], B.idf[:, :], r=["c_idf"], w=["c_idb"])
        B.blk = B.sb(st, "c_blk", [128, 128], F32)
        B.memset("dve", B.blk[:, :], 0.0, w=["c_blk"])
        B.memset("dve", B.blk[0:64, 0:64], 1.0, w=["c_blk"])
        B.memset("dve", B.blk[64:128, 64:128], 1.0, w=["c_blk"])
        B.epsc = B.sb(st, "c_eps", [128, 4], F32)
        for i, v in enumerate([1e-6, 1e-5, 64e-5, 1e-12]):
            B.memset("dve", B.epsc[:, i:i + 1], v, w=["c_eps"])
        B.eps6 = B.epsc[:, 0:1]
        I = B.I
        B.c_cwT = B.sb(st, "c_cwT", [128, 3, 31], F32)
        for c in range(3):
            B.ld(B.c_cwT[:, c, :], I["conv_w"][:, c * 128:(c + 1) * 128].rearrange("k p -> p k"), r=[], w=["c_cwT"], nc_ok=True, eng="pool", stream="c_cwT")
        B.c_vec3 = B.sb(st, "c_vec3", [128, 3, 3], F32)
        for j, nm in enumerate(["conv_b", "conv_ln_g", "conv_ln_b"]):
            B.ld(B.c_vec3[:, j, :], I[nm].rearrange("(c p) -> p c", p=128), r=[], w=["c_vec3"], nc_ok=True, eng="pool", stream="c_vec3")
        B.c_mu = B.sb(st, "c_mu", [128, 3, 12], F32)
        for j in range(2):
            B.ld(B.c_mu[:, 1 + j, :], I["shift_mu"][j, :].rearrange("(c p) -> p c", p=128), r=[], w=["c_mu"], nc_ok=True, eng="pool", stream="c_mu")

        def vecload(name, ap, shape):
            t = B.sb(st, name, shape, F32)
            B.ld(t[tuple(slice(None) for _ in shape)], ap, r=[], w=[name], nc_ok=True, eng="pool", stream=name)
            return t
        B.c_w0 = vecload("c_w0", I["decay_w0"].rearrange("d (c p) -> p d c", p=128), [128, 2, 3])
        B.c_a0 = vecload("c_a0", I["iclr_a0"].rearrange("d (c p) -> p d c", p=128), [128, 2, 3])
        B.c_kkv = vecload("c_kkv", I["k_k"].rearrange("(c p) -> p c", p=128), [128, 3])
        B.c_ka = vecload("c_ka", I["k_a"].rearrange("(c p) -> p c", p=128), [128, 3])
        B.c_rk = vecload("c_rk", I["r_k"].rearrange("(c p) -> p c", p=128), [128, 3])
        B.c_dup = vecload("c_dup", I["decay_up"], [128, 384])
        B.c_iup = vecload("c_iup", I["iclr_up"], [128, 384])
        B.c_gup = vecload("c_gup", I["gate_up"], [128, 384])
        B.c_vec6 = B.sb(st, "c_vec6", [128, 2, 3], F32)
        for j, nm in enumerate(["lnx_g", "lnx_b"]):
            B.ld(B.c_vec6[:, j, :], I[nm].rearrange("(c p) -> p c", p=128), r=[], w=["c_vec6"], nc_ok=True, eng="pool", stream="c_vec6")
        B.c_wr = vecload("c_wr", I["w_router"].rearrange("(kc p) n -> p kc n", p=128), [128, 8, E])

    def stage_inproj(self):
        B, P, I = self, self.P, self.I
        T = self.T
        with ExitStack() as st:
            W = B.sb(st, "s1_w", [128, 8, INP], BF16)
            wv = I["w_in"].rearrange("(kc p) n -> p kc n", p=128)
            for hh in range(2):
                B.ld(W[:, :, hh * 1280:(hh + 1) * 1280], wv[:, :, hh * 1280:(hh + 1) * 1280], r=[], w=["s1_w"],
                     eng="pool", stream="s1_w")
            gB = B.sb(st, "s1_g", [128, D], F32)
            B.ld(gB[:, :], I["norm_mix_g"].partition_broadcast(128), r=[], w=["s1_g"])
            xin = [B.sb(st, f"s1_x{i}", [128, D], F32) for i in range(2)]
            junk = B.sb(st, "s1_junk", [128, D], BF16)
            hb = [B.sb(st, f"s1_hb{i}", [128, D], BF16) for i in range(2)]
            hT = [B.sb(st, f"s1_hT{i}", [128, 8, 512], BF16) for i in range(2)]
            ss = [B.sb(st, f"s1_ss{i}", [128, 4], F32) for i in range(2)]
            og = [B.sb(st, f"s1_o{i}", [128, 5, 512], F32) for i in range(3)]
            pst = [B.ps(st, f"s1_pt{i}", [128, 8, 128], BF16) for i in range(2)]
            pm = [B.ps(st, f"s1_pm{i}", [128, 512], F32) for i in range(4)]
            nt = T // 512
            n128 = 0
            nmm = 0
            ngrp = 0
            for t5 in range(nt):
                hk = f"s1_hT{t5 % 2}"
                hTt = hT[t5 % 2]
                for sub in range(4):
                    i2 = n128 % 2
                    if n128 == 0:
                        B.ld(xin[0][:, :], I["x"][0:128, :], r=[], w=["s1_x0"])
                    n128 += 1
                    tok0 = t5 * 512 + sub * 128
                    if tok0 + 128 < T:
                        B.ld(xin[1 - i2][:, :], I["x"][tok0 + 128:tok0 + 256, :], r=[], w=[f"s1_x{1 - i2}"])
                    B.act(junk[:, :], xin[i2][:, :], AF.Square, r=[f"s1_x{i2}"], w=["s1_junk", f"s1_ss{i2}a"],
                          accum_out=ss[i2][:, 0:1])
                    B.act(ss[i2][:, 1:2], ss[i2][:, 0:1], AF.Sqrt, r=[f"s1_ss{i2}a"], w=[f"s1_ss{i2}b"],
                          bias=B.eps6, scale=1.0 / D)
                    B.P.op("dve", lambda e, o=ss[i2][:, 2:3], i=ss[i2][:, 1:2]: e.reciprocal(o, i),
                           r=[f"s1_ss{i2}b"], w=[f"s1_ss{i2}c"])
                    B.stt(hb[i2][:, :], xin[i2][:, :], ss[i2][:, 2:3], gB[:, :], OP.mult, OP.mult,
                          r=[f"s1_x{i2}", f"s1_ss{i2}c", "s1_g"], w=[f"s1_hb{i2}"])
                    for kc in range(8):
                        B.tr(pst[i2][:, kc, :], hb[i2][:, kc * 128:(kc + 1) * 128], B.idb[:, :],
                             r=[f"s1_hb{i2}", "c_idb"], w=[f"s1_pt{i2}"])
                    B.cp("act" if sub % 2 else "dve", hTt[:, :, sub * 128:(sub + 1) * 128], pst[i2][:, :, :],
                         r=[f"s1_pt{i2}"], w=[hk])
                for cg in range(4):
                    o = og[ngrp % 3]
                    ok = f"s1_o{ngrp % 3}"
                    ngrp += 1
                    for c5 in range(5):
                        c = cg * 5 + c5
                        pmi = nmm % 4
                        nmm += 1
                        for kc in range(8):
                            B.mm(pm[pmi][:, :], W[:, kc, c * 128:(c + 1) * 128], hTt[:, kc, :], kc == 0, kc == 7,
                                 r=["s1_w", hk], w=[f"s1_pm{pmi}"])
                        B.cp("act" if c % 2 else "dve", o[:, c5, :], pm[pmi][:, :], r=[f"s1_pm{pmi}"], w=[ok])
                    dst = self.pT[cg * 640:(cg + 1) * 640, t5 * 512:(t5 + 1) * 512].rearrange("(c p) t -> p c t", p=128)
                    B.st_dram(dst, o[:, :, :], r=[ok], w=[f"pT_{t5}_{cg}"], stream="st_" + ok)
        P.barrier()

    def rmsnorm_rows(self, xt, xk, gB, gk, out, outk, ss, ssk, junk, tmp=None, tmpk=None):
        B = self
        B.act(junk[:, :], xt, AF.Square, r=[xk], w=["junk", ssk + "a"], accum_out=ss[:, 0:1])
        B.act(ss[:, 1:2], ss[:, 0:1], AF.Sqrt, r=[ssk + "a"], w=[ssk + "b"], bias=B.eps6, scale=1.0 / D)
        B.P.op("dve", lambda e: e.reciprocal(ss[:, 2:3], ss[:, 1:2]), r=[ssk + "b"], w=[ssk + "c"])
        if tmp is None:
            B.stt(out, xt, ss[:, 2:3], gB, OP.mult, OP.mult, r=[xk, ssk + "c", gk], w=[outk])
        else:
            B.act(tmp, xt, AF.Copy, r=[xk, ssk + "c"], w=[tmpk], scale=ss[:, 2:3])
            B.tt("dve", out, tmp, gB, OP.mult, r=[tmpk, gk], w=[outk])

    def stage_attn(self):
        B, P, I = self, self.P, self.I
        S, NB = self.S, self.NB
        with ExitStack() as st:
            Wk = B.sb(st, "s2_wk", [128, 8, 256], BF16)
            Wv = B.sb(st, "s2_wv", [128, 8, 256], BF16)
            B.ld(Wk[:, :, :], I["w_mk"].rearrange("(kc p) n -> p kc n", p=128), r=[], w=["s2_wk"], eng="pool")
            B.ld(Wv[:, :, :], I["w_mv"].rearrange("(kc p) n -> p kc n", p=128), r=[], w=["s2_wv"], eng="pool")
            gB = B.sb(st, "s2_g", [128, D], F32)
            B.ld(gB[:, :], I["norm_mem_g"].partition_broadcast(128), r=[], w=["s2_g"])
            ON = B.sb(st, "s2_on", [128, 2, 128], BF16)
            B.memset("dve", ON[:, :, :], 0.0, w=["s2_on"])
            B.memset("dve", ON[:, 0, 0:64], 1.0, w=["s2_on"])
            B.memset("dve", ON[:, 1, 64:128], 1.0, w=["s2_on"])
            mt = B.sb(st, "s2_m", [128, D], F32)
            junk = B.sb(st, "s2_junk", [128, D], BF16)
            mb = B.sb(st, "s2_mb", [128, D], BF16)
            ss = B.sb(st, "s2_ss", [128, 4], F32)
            memT = B.sb(st, "s2_memT", [128, 8, 256], BF16)
            KT = B.sb(st, "s2_KT", [128, 2, 256], BF16)
            VA = B.sb(st, "s2_VA", [128, 2, 4, 128], BF16)
            qf = [B.sb(st, f"s2_qf{i}", [128, 2, 512], F32) for i in range(2)]
            qbs = [B.sb(st, f"s2_qb{i}", [128, 2, 512], BF16) for i in range(2)]
            PTs = [[B.sb(st, f"s2_PT{i}_{h}", [128, 2, 512], BF16) for h in range(4)] for i in range(2)]
            cnt2 = [0]
            rd = B.sb(st, "s2_rd", [128, 512], F32)
            ob = [B.sb(st, f"s2_ob{i}", [128, 2, 512], BF16) for i in range(2)]
            pst = B.ps(st, "s2_pt", [128, 8, 128], BF16)
            pa = [B.ps(st, f"s2_pa{i}", [128, 512], F32) for i in range(4)]
            pn = B.ps(st, "s2_pn", [128, 512], F32)
            pd = B.ps(st, "s2_pd", [128, 512], F32)
            npa = 0
            nq = 0
            for b in range(NB):
                for mc in range(2):
                    B.ld(mt[:, :], I["mem"][b * 256 + mc * 128: b * 256 + (mc + 1) * 128, :], r=[], w=["s2_m"])
                    B.rmsnorm_rows(mt[:, :], "s2_m", gB[:, :], "s2_g", mb[:, :], "s2_mb", ss, "s2_ss", junk)
                    for kc in range(8):
                        B.tr(pst[:, kc, :], mb[:, kc * 128:(kc + 1) * 128], B.idb[:, :], r=["s2_mb", "c_idb"], w=["s2_pt"])
                    B.cp("dve", memT[:, :, mc * 128:(mc + 1) * 128], pst[:, :, :], r=["s2_pt"], w=["s2_memT"])
                for hc in range(2):
                    p = pa[npa % 4]; pk = f"s2_pa{npa % 4}"; npa += 1
                    for kc in range(8):
                        B.mm(p[:, 0:256], Wk[:, kc, hc * 128:(hc + 1) * 128], memT[:, kc, :], kc == 0, kc == 7,
                             r=["s2_wk", "s2_memT"], w=[pk])
                    B.cp("act", KT[:, hc, :], p[:, 0:256], r=[pk], w=["s2_KT"])
                B.memset("pool", VA[:, :, :, :], 0.0, w=["s2_VA"])
                for mc in range(2):
                    p = pa[npa % 4]; pk = f"s2_pa{npa % 4}"; npa += 1
                    for kc in range(8):
                        B.mm(p[:, 0:256], memT[:, kc, mc * 128:(mc + 1) * 128], Wv[:, kc, :], kc == 0, kc == 7,
                             r=["s2_wv", "s2_memT"], w=[pk])
                    for h in range(4):
                        B.cp("dve", VA[:, mc, h, (h % 2) * 64:(h % 2) * 64 + 64], p[:, h * 64:(h + 1) * 64], r=[pk], w=["s2_VA"])
                NT2 = S // 512

                def scores(t5):
                    tok0 = b * S + t5 * 512
                    j = t5 % 2
                    q, qk = qf[j], f"s2_qf{j}"
                    B.ld(q[:, :, :], self.pT[0:256, tok0:tok0 + 512].rearrange("(c p) t -> p c t", p=128), r=[], w=[qk])
                    B.cp("dve", qbs[j][:, :, :], q[:, :, :], r=[qk], w=[f"s2_qb{j}"])
                    for h in range(4):
                        hc, base = h // 2, (h % 2) * 64
                        for mc in range(2):
                            cnt2[0] += 1
                            p = pa[cnt2[0] % 4]; pk = f"s2_pa{cnt2[0] % 4}"
                            B.mm(p[:, :], KT[base:base + 64, hc, mc * 128:(mc + 1) * 128], qbs[j][base:base + 64, hc, :], True, True,
                                 r=["s2_KT", f"s2_qb{j}"], w=[pk])
                            B.act(PTs[j][h][:, mc, :], p[:, :], AF.Exp, r=[pk], w=[f"s2_PT{j}_{h}"], scale=0.125)

                def pv(t5):
                    tok0 = b * S + t5 * 512
                    j = t5 % 2
                    o, okk = ob[j], f"s2_ob{j}"
                    for pr in range(2):
                        i = 0
                        for h in (2 * pr, 2 * pr + 1):
                            for mc in range(2):
                                B.mm(pn[:, :], VA[:, mc, h, :], PTs[j][h][:, mc, :], i == 0, i == 3, r=["s2_VA", f"s2_PT{j}_{h}"], w=["s2_pn"])
                                i += 1
                        i = 0
                        for h in (2 * pr, 2 * pr + 1):
                            for mc in range(2):
                                B.mm(pd[:, :], ON[:, h % 2, :], PTs[j][h][:, mc, :], i == 0, i == 3, r=["s2_on", f"s2_PT{j}_{h}"], w=["s2_pd"])
                                i += 1
                        B.P.op("dve", lambda e: e.reciprocal(rd[:, :], pd[:, :]), r=["s2_pd"], w=["s2_rd"])
                        B.tt("dve", o[:, pr, :], pn[:, :], rd[:, :], OP.mult, r=["s2_pn", "s2_rd"], w=[okk])
                    B.st_dram(self.mixT[0:256, tok0:tok0 + 512].rearrange("(c p) t -> p c t", p=128), o[:, :, :], r=[okk], w=[f"mixA_{tok0}"],
                              stream="st_" + okk, eng="pool")

                scores(0)
                for t5 in range(NT2):
                    if t5 + 1 < NT2:
                        scores(t5 + 1)
                    pv(t5)
        P.barrier()

    def stage_conv(self):
        B, P, I = self, self.P, self.I
        S, NB = self.S, self.NB
        with ExitStack() as st:
            cwT = B.c_cwT
            vec = B.c_vec3
            DG = B.sb(st, "s3_dg", [128, 3, 31, 128], BF16)
            for c in range(3):
                for k in range(31):
                    B.ts("dve", DG[:, c, k, :], B.idf[:, :], cwT[:, c, k:k + 1], OP.mult, r=["c_idf", "s3_cwT"], w=["s3_dg"])
            ones = B.sb(st, "s3_ones", [128, 128], F32)
            B.memset("dve", ones[:, :], 1.0 / 384.0, w=["s3_ones"])
            U = B.sb(st, "s3_U", [128, 3, S + 30], BF16)
            vf = [B.sb(st, f"s3_vf{i}", [128, 6, 512], F32) for i in range(2)]
            sg = B.sb(st, "s3_sg", [128, 3, 512], F32)
            Cf = B.sb(st, "s3_Cf", [128, 3, 512], F32)
            Cs = B.sb(st, "s3_Cs", [128, 3, 512], F32)
            mS = B.sb(st, "s3_mS", [128, 512], F32)
            m2 = B.sb(st, "s3_m2", [128, 512], F32)
            rs = B.sb(st, "s3_rs", [128, 512], F32)
            y = B.sb(st, "s3_y", [128, 512], F32)
            z = B.sb(st, "s3_z", [128, 512], F32)
            ob = [B.sb(st, f"s3_ob{i}", [128, 3, 512], BF16) for i in range(2)]
            pc = [B.ps(st, f"s3_pc{i}", [128, 512], F32) for i in range(3)]
            pmean = B.ps(st, "s3_pmean", [128, 512], F32)
            pmsq = B.ps(st, "s3_pmsq", [128, 512], F32)
            nv = [0]
            NT3 = S // 512
            B.memset("pool", U[:, :, 0:15], 0.0, w=["s3_Uh"])
            B.memset("pool", U[:, :, S + 15:S + 30], 0.0, w=["s3_Uh"])

            def uload(b, t5):
                tok0 = b * S + t5 * 512
                v = vf[t5 % 2]; vk = f"s3_vf{t5 % 2}"
                B.ld(v[:, :, :], self.pT[256:1024, tok0:tok0 + 512].rearrange("(c p) t -> p c t", p=128), r=[], w=[vk])

            def ubuild(b, t5):
                v = vf[t5 % 2]; vk = f"s3_vf{t5 % 2}"
                B.act(sg[:, :, :], v[:, 3:6, :], AF.Sigmoid, r=[vk], w=["s3_sg"])
                B.tt("dve", U[:, :, 15 + t5 * 512: 15 + (t5 + 1) * 512], v[:, 0:3, :], sg[:, :, :], OP.mult, r=[vk, "s3_sg"], w=[f"s3_U{t5}"])

            items3 = [(b, t5) for b in range(NB) for t5 in range(NT3)]
            for ii, (b, t5) in enumerate(items3):
                if t5 == 0:
                    uload(b, 0)
                    if NT3 > 1:
                        uload(b, 1)
                    ubuild(b, 0)
                    if NT3 > 2:
                        uload(b, 2)
                    if NT3 > 1:
                        ubuild(b, 1)
                if t5 + 3 < NT3:
                    uload(b, t5 + 3)
                if t5 + 2 < NT3:
                    ubuild(b, t5 + 2)
                if True:
                    tok0 = b * S + t5 * 512
                    o = ob[t5 % 2]; okk = f"s3_ob{t5 % 2}"
                    ukeys = [f"s3_U{j}" for j in (t5 - 1, t5, t5 + 1) if 0 <= j < NT3] + ["s3_Uh"]
                    for c in range(3):
                        for k in range(31):
                            B.mm(pc[c][:, :], DG[:, c, k, :], U[:, c, t5 * 512 + k: t5 * 512 + k + 512], k == 0, k == 30,
                                 r=["s3_dg"] + ukeys, w=[f"s3_pc{c}"])
                        B.act(Cf[:, c, :], pc[c][:, :], AF.Identity, r=[f"s3_pc{c}", "s3_vec"], w=["s3_Cf"], bias=vec[:, 0, c:c + 1])
                    B.tt("pool", Cs[:, :, :], Cf[:, :, :], Cf[:, :, :], OP.mult, r=["s3_Cf"], w=["s3_Cs"])
                    for c in range(3):
                        B.mm(pmean[:, :], ones[:, :], Cf[:, c, :], c == 0, c == 2, r=["s3_ones", "s3_Cf"], w=["s3_pmean"])
                    for c in range(3):
                        B.mm(pmsq[:, :], ones[:, :], Cs[:, c, :], c == 0, c == 2, r=["s3_ones", "s3_Cs"], w=["s3_pmsq"])
                    B.cp("act", mS[:, :], pmean[:, :], r=["s3_pmean"], w=["s3_mS"])
                    B.tt("pool", m2[:, :], mS[:, :], mS[:, :], OP.mult, r=["s3_mS"], w=["s3_m2"])
                    B.tt("dve", m2[:, :], pmsq[:, :], m2[:, :], OP.subtract, r=["s3_pmsq", "s3_m2"], w=["s3_m2"])
                    B.act(rs[:, :], m2[:, :], AF.Sqrt, r=["s3_m2"], w=["s3_rs"], bias=B.epsc[:, 1:2], scale=1.0)
                    B.P.op("dve", lambda e: e.reciprocal(rs[:, :], rs[:, :]), r=["s3_rs"], w=["s3_rs"])
                    for c in range(3):
                        B.tt("dve", y[:, :], Cf[:, c, :], mS[:, :], OP.subtract, r=["s3_Cf", "s3_mS"], w=["s3_y"])
                        B.tt("pool", y[:, :], y[:, :], rs[:, :], OP.mult, r=["s3_y", "s3_rs"], w=["s3_y"])
                        B.ts("dve", z[:, :], y[:, :], vec[:, 1, c:c + 1], OP.mult, r=["s3_y", "s3_vec"], w=["s3_z"], s2=vec[:, 2, c:c + 1], op1=OP.add)
                        B.act(y[:, :], z[:, :], AF.Sigmoid, r=["s3_z"], w=["s3_y"])
                        B.tt("dve", o[:, c, :], z[:, :], y[:, :], OP.mult, r=["s3_z", "s3_y"], w=[okk])
                    B.st_dram(self.mixT[256:640, tok0:tok0 + 512].rearrange("(c p) t -> p c t", p=128), o[:, :, :], r=[okk], w=[f"mixB_{tok0}"],
                              stream="st_" + okk, eng="pool")
        P.barrier()

    def stage_rwkv_prep(self):
        B, P, I = self, self.P, self.I
        S, NB = self.S, self.NB
        with ExitStack() as st:
            def vecload(name, ap, shape):
                t = B.sb(st, name, shape, F32)
                B.ld(t[tuple(slice(None) for _ in shape)], ap, r=[], w=[name], nc_ok=True)
                return t
            mu = B.c_mu
            B.tt("dve", mu[:, 0, :], mu[:, 1, :], mu[:, 2, :], OP.add, r=["s4_mu"], w=["s4_mu"])
            B.ts("dve", mu[:, 0, :], mu[:, 0, :], -1.0, OP.mult, r=["s4_mu"], w=["s4_mu"], s2=1.0, op1=OP.add)
            w0, a0, kkv, ka, rk = B.c_w0, B.c_a0, B.c_kkv, B.c_ka, B.c_rk
            omka = B.sb(st, "s4_omka", [128, 3], F32)
            B.ts("dve", omka[:, :], ka[:, :], -1.0, OP.mult, r=["s4_ka"], w=["s4_omka"], s2=1.0, op1=OP.add)
            dup, iup, gup = B.c_dup, B.c_iup, B.c_gup
            rmask = B.sb(st, "s4_rmask", [128, 512], F32)
            B.memset("dve", rmask[:, :], 1.0, w=["s4_rmask"])
            B.memset("dve", rmask[:, :].rearrange("p (n s) -> p n s", s=64)[:, :, 0:1], 0.0, w=["s4_rmask"])
            L = B.sb(st, "s4_L", [128, 12, 514], F32)
            pfs = [B.sb(st, f"s4_pf{i}", [128, 12, 512], F32) for i in range(2)]
            TW = [B.sb(st, f"s4_tw{i}", [128, 512], F32) for i in range(2)]
            SG = [B.sb(st, f"s4_sgd{i}", [128, 512], F32) for i in range(2)]
            names = ["Gc", "kq", "sq", "kk", "sw", "a", "kd0", "kd1", "bb", "pre", "cum", "ex", "Ec", "Ei", "Ex", "t1"]
            alias = {"sd": "sq", "lw": "sw", "tmp": "t1", "bon": "Gc", "rt": "Ec", "at": "Ex", "bt": "bb", "kt": "Ei"}
            Xs = [{n: B.sb(st, f"s4_{n}_{i}", [128, 512], F32) for n in names} for i in range(3)]
            cur = [0]

            class _XV:
                def __getitem__(self, n):
                    return Xs[cur[0]][alias.get(n, n)]
            X = _XV()
            PLt = B.sb(st, "s4_PLt", [128, 8], F32)
            TMs = [B.sb(st, f"s4_TMs{i}", [128, 4, 128], F32) for i in range(2)]
            ps = [B.ps(st, f"s4_ps{i}", [128, 512], F32) for i in range(6)]
            pst = [B.ps(st, f"s4_pst{i}", [128, 4, 128], F32) for i in range(2)]
            cnt = {"ps": 0, "pst": 0}

            def K(n):
                return f"s4_{alias.get(n, n)}_{cur[0]}"

            def nps():
                i = cnt["ps"] % 6; cnt["ps"] += 1
                return ps[i], f"s4_ps{i}"

            def transpose_store(src, dst_ap, dkey):
                i = cnt["pst"] % 2; cnt["pst"] += 1
                for sub in range(4):
                    B.tr(pst[i][:, sub, :], X[src][:, sub * 128:(sub + 1) * 128], B.idf[:, :], r=[K(src), "c_idf"], w=[f"s4_pst{i}"])
                B.cp("act" if i else "dve", TMs[i][:, :, :], pst[i][:, :, :], r=[f"s4_pst{i}"], w=[f"s4_TMs{i}"])
                B.st_dram(dst_ap, TMs[i][:, :, :], r=[f"s4_TMs{i}"], w=[dkey])

            NEG = -0.606531
            active = []
            ntile = [0]

            def step_all():
                for g in list(active):
                    cur[0] = g[1]
                    try:
                        next(g[0])
                    except StopIteration:
                        active.remove(g)

            def launch(gen):
                while len(active) >= 3:
                    step_all()
                used = {g[1] for g in active}
                si = [i for i in range(3) if i not in used][0]
                active.append([gen, si])

            for b in range(NB):
                for t5 in range(S // 512):
                    tok0 = b * S + t5 * 512
                    tp = ntile[0] % 2
                    ntile[0] += 1
                    pf = pfs[tp]
                    pfk = (lambda j, tp=tp: f"s4_pf{tp}_{j}")
                    tw, sgd, twk, sgk = TW[tp], SG[tp], f"s4_tw{tp}", f"s4_sgd{tp}"
                    def loadL(b_, t5_):
                        tk0 = b_ * S + t5_ * 512
                        lo = 1 if t5_ == 0 else 0
                        hi = 513 if t5_ == S // 512 - 1 else 514
                        if lo:
                            B.memset("pool", L[:, :, 0:1], 0.0, w=["s4_L"])
                        if hi == 513:
                            B.memset("pool", L[:, :, 513:514], 0.0, w=["s4_L"])
                        for half in range(2):
                            B.ld(L[:, half * 6:(half + 1) * 6, lo:hi],
                                 self.pT[1024 + half * 768:1024 + (half + 1) * 768, tk0 - 1 + lo: tk0 - 1 + hi].rearrange("(c p) t -> p c t", p=128),
                                 r=[], w=["s4_L"], stream="s4_L")
                    if b == 0 and t5 == 0:
                        loadL(0, 0)
                    for c in range(12):
                        B.act(pf[:, c, :], L[:, c, 1:513], AF.Copy, r=["s4_L", "s4_mu"], w=[pfk(c)], scale=mu[:, 0, c:c + 1])
                        B.stt(pf[:, c, :], L[:, c, 0:512], mu[:, 1, c:c + 1], pf[:, c, :], OP.mult, OP.add, r=["s4_L", "s4_mu", pfk(c)], w=[pfk(c)])
                        B.stt(pf[:, c, :], L[:, c, 2:514], mu[:, 2, c:c + 1], pf[:, c, :], OP.mult, OP.add, r=["s4_L", "s4_mu", pfk(c)], w=[pfk(c)])
                    nxt = b * (S // 512) + t5 + 1
                    if nxt < NB * (S // 512):
                        loadL(nxt // (S // 512), nxt % (S // 512))
                    B.act(tw[:, :], pf[:, 9, :], AF.Tanh, r=[pfk(9)], w=[twk])
                    B.act(sgd[:, :], pf[:, 11, :], AF.Sigmoid, r=[pfk(11)], w=[sgk])
                    def citer(c, tok0=tok0, pf=pf, pfk=pfk, tw=tw, sgd=sgd, twk=twk, sgk=sgk):
                        cs = slice(c * 128, (c + 1) * 128)
                        rT, kT, vT = pf[:, c, :], pf[:, 3 + c, :], pf[:, 6 + c, :]
                        rK, kK, vK = pfk(c), pfk(3 + c), pfk(6 + c)
                        p, pk = nps()
                        B.mm(p[:, :], gup[:, cs], sgd[:, :], True, True, r=["s4_gup", sgk], w=[pk])
                        B.cp("act", X["Gc"][:, :], p[:, :], r=[pk], w=[K("Gc")])
                        B.st_dram(self.gT[cs, tok0:tok0 + 512], X["Gc"][:, :], r=[K("Gc")], w=[f"gT{tok0}_{c}"])
                        B.act(X["kq"][:, :], kT, AF.Copy, r=[kK, "s4_kkv"], w=[K("kq")], scale=kkv[:, c:c + 1])
                        B.act(X["sq"][:, :], kT, AF.Square, r=[kK, "s4_kkv"], w=[K("sq")], scale=kkv[:, c:c + 1])
                        yield
                        p, pk = nps()
                        B.mm(p[:, :], B.blk[:, :], X["sq"][:, :], True, True, r=["c_blk", K("sq")], w=[pk])
                        B.act(X["sd"][:, :], p[:, :], AF.Sqrt, r=[pk], w=[K("sd")], bias=B.epsc[:, 3:4], scale=1.0)
                        yield
                        B.P.op("dve", lambda e, t=X["sd"]: e.reciprocal(t[:, :], t[:, :]), r=[K("sd")], w=[K("sd")])
                        B.tt("dve", X["kk"][:, :], X["kq"][:, :], X["sd"][:, :], OP.mult, r=[K("kq"), K("sd")], w=[K("kk")])
                        yield
                        for d in range(2):
                            ds = slice(d * 64, (d + 1) * 64)
                            kd = f"kd{d}"
                            p, pk = nps()
                            B.mm(p[:, :], dup[ds, cs], tw[ds, :], True, True, r=["s4_dup", twk], w=[pk])
                            B.act(X["sw"][:, :], p[:, :], AF.Sigmoid, r=[pk, "s4_w0"], w=[K("sw")], bias=w0[:, d, c:c + 1], scale=1.0)
                            B.act(X["lw"][:, :], X["sw"][:, :], AF.Copy, r=[K("sw")], w=[K("lw")], scale=NEG)
                            yield
                            p, pk = nps()
                            B.mm(p[:, :], iup[ds, cs], pf[ds, 10, :], True, True, r=["s4_iup", pfk(10)], w=[pk])
                            B.act(X["a"][:, :], p[:, :], AF.Sigmoid, r=[pk, "s4_a0"], w=[K("a")], bias=a0[:, d, c:c + 1], scale=1.0)
                            yield
                            B.ts("dve", X["tmp"][:, :], X["a"][:, :], ka[:, c:c + 1], OP.mult, r=[K("a"), "s4_ka", "s4_omka"], w=[K("tmp")],
                                 s2=omka[:, c:c + 1], op1=OP.add)
                            B.tt("dve", X[kd][:, :], kT, X["tmp"][:, :], OP.mult, r=[kK, K("tmp")], w=[K(kd)])
                            B.tt("dve", X["bb"][:, :], X["kk"][:, :], X["a"][:, :], OP.mult, r=[K("kk"), K("a")], w=[K("bb")])
                            yield
                            B.P.op("dve", lambda e, t0=X["pre"], t1=X["lw"]: e.tensor_tensor_scan(t0[:, :], rmask[:, :], t1[:, :], 0.0, OP.mult, OP.add),
                                   r=["s4_rmask", K("lw")], w=[K("pre")])
                            yield
                            if d == 0:
                                cumk = "pre"
                                B.tt("dve", X["ex"][:, :], X["pre"][:, :], X["lw"][:, :], OP.subtract, r=[K("pre"), K("lw")], w=[K("ex")])
                            else:
                                cumk = "cum"
                                tot = X["pre"][:, :].rearrange("p (n s) -> p n s", s=64)[:, :, 63:64].to_broadcast([128, 8, 64])
                                B.tt("dve", X["ex"][:, :].rearrange("p (n s) -> p n s", s=64), tot,
                                     X["pre"][:, :].rearrange("p (n s) -> p n s", s=64), OP.subtract, r=[K("pre")], w=[K("ex")])
                                B.tt("dve", X["cum"][:, :], X["ex"][:, :], X["lw"][:, :], OP.add, r=[K("ex"), K("lw")], w=[K("cum")])
                            B.act(X["Ec"][:, :], X[cumk][:, :], AF.Exp, r=[K(cumk)], w=[K("Ec")])
                            B.act(X["Ei"][:, :], X[cumk][:, :], AF.Exp, r=[K(cumk)], w=[K("Ei")], scale=-1.0)
                            B.act(X["Ex"][:, :], X["ex"][:, :], AF.Exp, r=[K("ex")], w=[K("Ex")])
                            yield
                            B.stt(X["at"][:, :], X["kk"][:, :], -1.0, X["Ex"][:, :], OP.mult, OP.mult, r=[K("kk"), K("Ex")], w=[K("at")])
                            B.tt("dve", X["bt"][:, :], X["bb"][:, :], X["Ei"][:, :], OP.mult, r=[K("bb"), K("Ei")], w=[K("bt")])
                            B.tt("dve", X["kt"][:, :], X[kd][:, :], X["Ei"][:, :], OP.mult, r=[K(kd), K("Ei")], w=[K("kt")])
                            yield
                            pos = 63 if d == 0 else 0
                            B.cp("act", PLt[:, :], X["Ec"][:, :].rearrange("p (n s) -> p n s", s=64)[:, :, pos], r=[K("Ec")], w=["s4_PLt"])
                            B.tt("pool", X["rt"][:, :], rT, X["Ec"][:, :], OP.mult, r=[rK, K("Ec")], w=[K("rt")])
                            B.st_dram(self.PLd[d][cs, tok0 // 64: tok0 // 64 + 8], PLt[:, :], r=["s4_PLt"], w=[f"PL{d}_{tok0}_{c}"])
                            for qi, q in enumerate(["rt", "at", "bt", "kt"]):
                                B.st_dram(self.FM[d][qi, cs, tok0:tok0 + 512], X[q][:, :], r=[K(q)], w=[f"FM{d}_{qi}_{tok0}_{c}"])
                            for qi, q in enumerate(["at", "bt", "kt"]):
                                transpose_store(q, self.TM[d][tok0:tok0 + 512, qi, cs].rearrange("(s p) ch -> p s ch", p=128), f"TM{d}_{qi}_{tok0}_{c}")
                                yield
                        B.tt("pool", X["t1"][:, :], X["kd0"][:, :], X["kd1"][:, :], OP.add, r=[K("kd0"), K("kd1")], w=[K("t1")])
                        B.tt("pool", X["t1"][:, :], X["t1"][:, :], rT, OP.mult, r=[K("t1"), rK], w=[K("t1")])
                        B.act(X["t1"][:, :], X["t1"][:, :], AF.Copy, r=[K("t1"), "s4_rk"], w=[K("t1")], scale=rk[:, c:c + 1])
                        yield
                        p, pk = nps()
                        B.mm(p[:, :], B.blk[:, :], X["t1"][:, :], True, True, r=["c_blk", K("t1")], w=[pk])
                        B.tt("dve", X["bon"][:, :], p[:, :], vT, OP.mult, r=[pk, vK], w=[K("bon")])
                        B.st_dram(self.bonT[cs, tok0:tok0 + 512], X["bon"][:, :], r=[K("bon")], w=[f"bonT{tok0}_{c}"])
                        yield
                        i = cnt["pst"] % 2; cnt["pst"] += 1
                        for sub in range(4):
                            B.tr(pst[i][:, sub, :], pf[:, 6 + c, sub * 128:(sub + 1) * 128], B.idf[:, :], r=[vK, "c_idf"], w=[f"s4_pst{i}"])
                        B.cp("act" if i else "dve", TMs[i][:, :, :], pst[i][:, :, :], r=[f"s4_pst{i}"], w=[f"s4_TMs{i}"])
                        B.st_dram(self.TMV[tok0:tok0 + 512, cs].rearrange("(s p) ch -> p s ch", p=128), TMs[i][:, :, :], r=[f"s4_TMs{i}"], w=[f"TMV_{tok0}_{c}"])
                    for c in range(3):
                        launch(citer(c))
            while active:
                step_all()
        P.barrier()

    def stage_rwkv_scan(self):
        B, P, I = self, self.P, self.I
        S, NB = self.S, self.NB
        NP = S // 128
        with ExitStack() as st:
            MK = B.sb(st, "s5_mk", [128, 4, 128], F32)
            for i, op in enumerate([OP.is_ge, OP.is_gt, OP.is_le, OP.is_lt]):
                B.ts("dve", MK[:, i, :], B.dif[:, :], 0.0, op, r=["c_dif"], w=["s5_mk"])
                B.tt("dve", MK[:, i, :], MK[:, i, :], B.blk[:, :], OP.mult, r=["s5_mk", "c_blk"], w=["s5_mk"])
            FMt = [B.sb(st, f"s5_FM{i}", [64, 6, 4, 128], F32) for i in range(2)]
            TMt = [B.sb(st, f"s5_TM{i}", [128, 4 * 384 + 64], F32) for i in range(2)]
            H = [[B.sb(st, f"s5_H{d}_{h}", [64, 128], F32) for h in range(6)] for d in range(2)]
            BT = [B.sb(st, f"s5_BT_{h}", [128, 512], F32) for h in range(6)]
            E2 = [B.sb(st, f"s5_E2_{h}", [128, 256], F32) for h in range(6)]
            RhT = [B.sb(st, f"s5_Rh_{h}", [64, 128], F32) for h in range(6)]
            MT = [B.sb(st, f"s5_MT_{h}", [64, 2, 128], F32) for h in range(6)]
            Gp = [B.sb(st, f"s5_Gp_{h}", [64, 2, 64], F32) for h in range(6)]
            Y0 = [B.sb(st, f"s5_Y0_{h}", [128, 128], F32) for h in range(6)]
            YT = [B.sb(st, f"s5_YT_{h}", [128, 128], F32) for h in range(6)]
            PS = [B.ps(st, f"s5_ps{h}", [128, 512], F32) for h in range(6)]
            R = mybir.dt.float32r
            FMr = [B.sb(st, f"s5_FMr{i}", [64, 6, 4, 128], F32) for i in range(2)]
            TMr = [B.sb(st, f"s5_TMr{i}", [128, 3 * 384 + 64], F32) for i in range(2)]
            VZ = [B.sb(st, f"s5_VZ{i}", [128, 6, 128], F32) for i in range(2)]
            I2 = B.sb(st, "s5_i2", [64, 2, 64], F32)
            for c in range(2):
                B.cp("dve", I2[:, c, :], B.idf[0:64, 0:64], r=["c_idf"], w=["s5_i2"])
            ZR = B.sb(st, "s5_zr", [128, 768], F32)
            B.memset("dve", ZR[:, :], 0.0, w=["s5_zr"])
            Bbd = [B.sb(st, f"s5_Bbd{i}", [128, 6, 128], F32) for i in range(2)]
            Vbd = [B.sb(st, f"s5_Vbd{i}", [128, 6, 128], F32) for i in range(2)]
            W0bd = [B.sb(st, f"s5_W0bd{h}", [128, 128], F32) for h in range(6)]
            for i in range(2):
                B.cp("dve", Bbd[i][:, :, :].bitcast(R), ZR[:, :].rearrange("p (h i) -> p h i", i=128), r=["s5_zr"], w=[f"s5_Bbd{i}"])
                B.cp("dve", Vbd[i][:, :, :].bitcast(R), ZR[:, :].rearrange("p (h i) -> p h i", i=128), r=["s5_zr"], w=[f"s5_Vbd{i}"])
            for h in range(6):
                B.cp("dve", W0bd[h][:, :].bitcast(R), ZR[:, 0:128], r=["s5_zr"], w=[f"W0bd{h}"])
            for i in range(2):
                B.cp("dve", VZ[i][:, :, :].bitcast(R), ZR[:, :].rearrange("p (h i) -> p h i", i=128), r=["s5_zr"], w=[f"s5_VZ{i}"])
                B.cp("dve", TMr[i][:, 3 * 384:].bitcast(R), ZR[:, 0:64], r=["s5_zr"], w=[f"s5_TMr{i}"])
            for i in range(2):
                B.memset("pool", TMt[i][:, 4 * 384:], 0.0, w=[f"s5_TM{i}"])

            import os
            STOP = int(os.environ.get('CHAIN_STOP', '0'))

            def chain(b, d, p, h, gi):
                fm, tmf, fmo = FMr[gi], TMr[gi], FMt[gi]
                vz = VZ[gi]

                def tmq(rows, q, width=64):
                    if q == 3:
                        return vz[rows, h, 64:128].bitcast(R)
                    return tmf[rows, q * 384 + h * 64: q * 384 + h * 64 + width].bitcast(R)
                al = slice(0, 128)
                fk, tk, vk = f"s5_FMr{gi}", f"s5_TMr{gi}", f"s5_VZ{gi}"
                ps, pk = PS[h], f"s5_ps{h}"
                bt, e2 = BT[h], E2[h]
                kQT, kArb, kE2, kQ, kZ = f"QT{h}", f"Arb{h}", f"E2{h}", f"Q{h}", f"Z{h}"
                hs = slice(h * 64, (h + 1) * 64)
                mT = MK[:, 0:2, :] if d == 0 else MK[:, 2:4, :]
                mN = MK[:, 3, :] if d == 0 else MK[:, 1, :]
                tok0 = b * S + p * 128
                B.mm(ps[:, 0:256], fm[:, h, 2, :].bitcast(R), fm[:, h, 0:2, :].bitcast(R), True, True, r=[fk], w=[pk])
                B.mm(ps[:, 256:512], fm[:, h, 3, :].bitcast(R), fm[:, h, 0:2, :].bitcast(R), True, True, r=[fk], w=[pk])
                yield
                if STOP == 1: return
                B.tt("dve", bt[:, 0:256].bitcast(R).rearrange("p (a s) -> p a s", a=2), ps[:, 0:256].rearrange("p (a s) -> p a s", a=2), mT, OP.mult,
                     r=[pk, "s5_mk"], w=[kQT, kArb])
                B.tt("dve", e2[:, :].bitcast(R).rearrange("p (a s) -> p a s", a=2), ps[:, 256:512].rearrange("p (a s) -> p a s", a=2), mT, OP.mult,
                     r=[pk, "s5_mk"], w=[kE2])
                yield
                if STOP == 2: return
                B.mm(ps[:, 0:128], fm[:, h, 1, :].bitcast(R), fm[:, h, 2, :].bitcast(R), True, True, r=[fk], w=[pk])
                B.mm(ps[:, 128:192], e2[:, 128:256].bitcast(R), tmq(al, 3), True, True, r=[kE2, vk], w=[pk])
                yield
                if STOP == 3: return
                B.tt("dve", bt[:, 256:384].bitcast(R), ps[:, 0:128], mN, OP.mult, r=[pk, "s5_mk"], w=[pk, kQ])
                B.cp("act", bt[:, 448:512].bitcast(R), ps[:, 128:192], r=[pk], w=[pk, kZ])
                B.cp("act", bt[:, 384:448].bitcast(R), tmf[:, h * 64:(h + 1) * 64], r=[tk], w=[kZ])
                yield
                if STOP == 4: return
                for k in range(6):
                    if k < 5:
                        B.mm(ps[:, 128:384], bt[:, 128:256].bitcast(R), bt[:, 256:512].bitcast(R), True, True, r=[kQT, kQ, kZ], w=[pk])
                        B.mm(ps[:, 0:128], bt[:, 256:384].bitcast(R), bt[:, 128:256].bitcast(R), True, True, r=[kQT, kQ], w=[pk])
                    else:
                        B.mm(ps[:, 256:384], bt[:, 128:256].bitcast(R), bt[:, 384:512].bitcast(R), True, True, r=[kQT, kZ], w=[pk])
                    yield
                    if STOP == 5: return
                    B.tt("dve", bt[:, 384:512].bitcast(R), ps[:, 256:384], bt[:, 384:512], OP.add, r=[pk, kZ], w=[pk, kZ])
                    if k < 5:
                        B.cp("act", bt[:, 128:384].bitcast(R), ps[:, 0:256], r=[pk], w=[pk, kQ, kQT])
                    yield
                    if STOP == 6: return
                for c in range(2):
                    cr = slice(c * 64, (c + 1) * 64)
                    B.cp("act", W0bd[h][cr, c * 64:(c + 1) * 64].bitcast(R), bt[cr, 448:512], r=[kZ], w=[f"W0bd{h}"])
                B.mm(ps[:, 0:128], bt[:, 384:512].bitcast(R), bt[:, 0:128].bitcast(R), True, False, r=[kZ, kArb], w=[pk])
                B.mm(ps[:, 0:128], vz[:, h, :].bitcast(R), e2[:, 0:128].bitcast(R), False, True, r=[vk, kE2], w=[pk])
                B.mm(ps[:, 128:256], bt[:, 384:512].bitcast(R), Bbd[gi][:, h, :].bitcast(R), True, True, r=[kZ, f"s5_Bbd{gi}"], w=[pk])
                B.mm(ps[:, 256:384], tmq(al, 1, 128), W0bd[h][:, :].bitcast(R), True, False, r=[tk, f"W0bd{h}"], w=[pk])
                B.mm(ps[:, 256:384], tmq(al, 2, 128), Vbd[gi][:, h, :].bitcast(R), False, True, r=[tk, f"s5_Vbd{gi}"], w=[pk])
                yield
                if STOP == 7: return
                B.tt("dve", RhT[h][:, :].bitcast(R), ps[0:64, 0:128], fmo[:, h, 0, :], OP.add, r=[pk, f"s5_FM{gi}"], w=[pk, f"Rh{h}"])
                B.tt("dve", MT[h][:, :, 0:64].bitcast(R), ps[0:64, 128:256].rearrange("p (c j) -> p c j", c=2), I2[:, :, :], OP.add, r=[pk, "s5_i2"], w=[pk, f"MT{h}"])
                for c in range(2):
                    n = p * 2 + c
                    B.act(Gp[h][:, c, :], ps[0:64, 256 + c * 64:320 + c * 64], AF.Copy, r=[pk, f"s5_PL{b}_{d}"], w=[pk, f"Gp{h}"], scale=PLtb[b][d][:, h, n:n + 1])
                B.cp("act", Y0[h][64:128, :], ps[64:128, 0:128], r=[pk], w=[pk, f"Y0{h}"])
                yield
                if STOP == 8: return
                Hs, hk = H[d][h], f"H{d}_{h}"
                for c in ((0, 1) if d == 0 else (1, 0)):
                    n = p * 2 + c
                    cc = slice(c * 64, (c + 1) * 64)
                    B.mm(ps[:, 0:64], Hs[:, :].bitcast(R), RhT[h][:, cc].bitcast(R), True, True, r=[hk, f"Rh{h}"], w=[pk])
                    B.mm(ps[:, 64:128], MT[h][:, c, :].bitcast(R), Hs[:, 64:128].bitcast(R), True, True, r=[hk, f"MT{h}"], w=[pk])
                    yield
                    if STOP == 9: return
                    B.tt("dve", YT[h][64:128, cc], ps[64:128, 0:64], Y0[h][64:128, cc], OP.add, r=[pk, f"Y0{h}"], w=[pk, f"YT{h}"])
                    B.stt(Hs[:, 64:128].bitcast(R), ps[0:64, 64:128], PLtb[b][d][:, h, n:n + 1], Gp[h][:, c, :], OP.mult, OP.add,
                          r=[pk, f"s5_PL{b}_{d}", f"Gp{h}"], w=[pk, hk])
                    yield
                    if STOP == 10: return
                B.st_dram(self.YT[d][hs, tok0:tok0 + 128], YT[h][64:128, :], r=[f"YT{h}"], w=[f"YTd{d}_{h}_{tok0}"], stream=f"st_YT{h}")

            groups = []
            for b in range(NB):
                for i in range(NP):
                    for d in range(2):
                        groups.append((b, d, i if d == 0 else NP - 1 - i))

            def prologue(g):
                b, d, p = groups[g]
                gi = g % 2
                tok0 = b * S + p * 128
                for q in range(4):
                    B.ld(FMt[gi][:, :, q, :], self.FM[d][q, :, tok0:tok0 + 128].rearrange("(h j) t -> j h t", j=64), r=[], w=[f"s5_FM{gi}"],
                         stream=f"s5_FM{gi}")
                B.ld(TMt[gi][:, 0:3 * 384], self.TM[d][tok0:tok0 + 128, :, :].rearrange("t q c -> t (q c)"), r=[], w=[f"s5_TM{gi}"], stream=f"s5_TM{gi}")
                B.ld(TMt[gi][:, 3 * 384:4 * 384], self.TMV[tok0:tok0 + 128, :], r=[], w=[f"s5_TM{gi}"], stream=f"s5_TM{gi}")
                B.cp("pool", FMr[gi][:, :, :, :].bitcast(R), FMt[gi][:, :, :, :], r=[f"s5_FM{gi}"], w=[f"s5_FMr{gi}"])
                B.cp("pool", TMr[gi][:, 0:3 * 384].bitcast(R), TMt[gi][:, 0:3 * 384], r=[f"s5_TM{gi}"], w=[f"s5_TMr{gi}"])
                B.cp("pool", VZ[gi][:, :, 64:128].bitcast(R), TMt[gi][:, 3 * 384:4 * 384].rearrange("p (h i) -> p h i", i=64), r=[f"s5_TM{gi}"], w=[f"s5_VZ{gi}"])
                for c in range(2):
                    cr = slice(c * 64, (c + 1) * 64)
                    B.cp("pool", Bbd[gi][cr, :, c * 64:(c + 1) * 64].bitcast(R), TMt[gi][cr, 384:768].rearrange("p (h i) -> p h i", i=64),
                         r=[f"s5_TM{gi}"], w=[f"s5_Bbd{gi}"])
                    B.cp("pool", Vbd[gi][cr, :, c * 64:(c + 1) * 64].bitcast(R), TMt[gi][cr, 3 * 384:4 * 384].rearrange("p (h i) -> p h i", i=64),
                         r=[f"s5_TM{gi}"], w=[f"s5_Vbd{gi}"])

            PLtb = [[B.sb(st, f"s5_PLb{b}_{d}", [64, 6, S // 64], F32) for d in range(2)] for b in range(NB)]
            for b in range(NB):
                for d in range(2):
                    B.ld(PLtb[b][d][:, :, :], self.PLd[d][:, b * (S // 64):(b + 1) * (S // 64)].rearrange("(h j) c -> j h c", j=64), r=[], w=[f"s5_PL{b}_{d}"])
            prologue(0)
            for g, (b, d, p) in enumerate(groups):
                if p == (0 if d == 0 else NP - 1):
                    for h in range(6):
                        B.cp("dve", H[d][h][:, :].bitcast(R), ZR[0:64, 0:128], r=["s5_zr"], w=[f"H{d}_{h}"])
                        if g == 0:
                            B.cp("dve", MT[h][:, :, :].bitcast(R), ZR[0:64, 0:256].rearrange("p (c j) -> p c j", c=2), r=["s5_zr"], w=[f"MT{h}"])
                if g + 1 < len(groups):
                    prologue(g + 1)
                gens = [chain(b, d, p, h, g % 2) for h in range(6)]
                while gens:
                    for gg in list(gens):
                        try:
                            next(gg)
                        except StopIteration:
                            gens.remove(gg)
        P.barrier()

    def stage_rwkv_post(self):
        B, P, I = self, self.P, self.I
        S, NB = self.S, self.NB
        with ExitStack() as st:
            vec = B.c_vec6
            bm = B.sb(st, "s6_bm", [128, 128], F32)
            B.ts("dve", bm[:, :], B.blk[:, :], 1.0 / 64.0, OP.mult, r=["c_blk"], w=["s6_bm"])
            names = ["yf", "yb", "g", "bon", "y", "sq", "mS", "m2", "rs", "z"]
            NS6 = 3
            Xs = [{n: B.sb(st, f"s6_{n}{i}", [128, 512], F32) for n in names} for i in range(NS6)]
            ob = [B.sb(st, f"s6_ob{i}", [128, 512], BF16) for i in range(NS6)]
            p1 = [B.ps(st, f"s6_p1{i}", [128, 512], F32) for i in range(NS6)]
            p2 = [B.ps(st, f"s6_p2{i}", [128, 512], F32) for i in range(NS6)]
            its = [(tok0, c) for tok0 in range(0, NB * S, 512) for c in range(3)]

            def comp(i, j):
                tok0, c = its[i]
                X = Xs[j]
                cs = slice(c * 128, (c + 1) * 128)
                ts_ = slice(tok0, tok0 + 512)
                K = lambda n: f"s6_{n}{j}"
                for n, src in (("yf", self.YT[0]), ("yb", self.YT[1]), ("g", self.gT), ("bon", self.bonT)):
                    B.ld(X[n][:, :], src[cs, ts_], r=[], w=[K(n)])
                yield
                P1, P2, k1, k2 = p1[j], p2[j], f"s6_p1{j}", f"s6_p2{j}"
                B.tt("pool", X["y"][:, :], X["yf"][:, :], X["yb"][:, :], OP.add, r=[K("yf"), K("yb")], w=[K("y")])
                yield
                B.tt("dve", X["sq"][:, :], X["y"][:, :], X["y"][:, :], OP.mult, r=[K("y")], w=[K("sq")])
                B.mm(P1[:, :], bm[:, :], X["y"][:, :], True, True, r=["s6_bm", K("y")], w=[k1])
                yield
                B.mm(P2[:, :], bm[:, :], X["sq"][:, :], True, True, r=["s6_bm", K("sq")], w=[k2])
                B.cp("act", X["mS"][:, :], P1[:, :], r=[k1], w=[K("mS")])
                yield
                B.act(X["m2"][:, :], X["mS"][:, :], AF.Square, r=[K("mS")], w=[K("m2")])
                B.tt("dve", X["y"][:, :], X["y"][:, :], X["mS"][:, :], OP.subtract, r=[K("y"), K("mS")], w=[K("y")])
                yield
                B.tt("dve", X["m2"][:, :], P2[:, :], X["m2"][:, :], OP.subtract, r=[k2, K("m2")], w=[K("m2")])
                yield
                B.act(X["rs"][:, :], X["m2"][:, :], AF.Sqrt, r=[K("m2")], w=[K("rs")], bias=B.epsc[:, 2:3], scale=1.0)
                yield
                B.P.op("dve", lambda e, t=X["rs"]: e.reciprocal(t[:, :], t[:, :]), r=[K("rs")], w=[K("rs")])
                B.tt("dve", X["y"][:, :], X["y"][:, :], X["rs"][:, :], OP.mult, r=[K("y"), K("rs")], w=[K("y")])
                B.ts("dve", X["z"][:, :], X["y"][:, :], vec[:, 0, c:c + 1], OP.mult, r=[K("y"), "s6_vec"], w=[K("z")], s2=vec[:, 1, c:c + 1], op1=OP.add)
                yield
                B.tt("pool", X["z"][:, :], X["z"][:, :], X["bon"][:, :], OP.add, r=[K("z"), K("bon")], w=[K("z")])
                yield
                o, okk = ob[j], f"s6_ob{j}"
                B.tt("dve", o[:, :], X["z"][:, :], X["g"][:, :], OP.mult, r=[K("z"), K("g")], w=[okk])
                B.st_dram(self.mixT[640 + c * 128: 640 + (c + 1) * 128, ts_], o[:, :], r=[okk], w=[f"mixC{tok0}_{c}"], eng="pool")

            active = []

            def step_all():
                for g in list(active):
                    try:
                        next(g[0])
                    except StopIteration:
                        active.remove(g)

            for i in range(len(its)):
                while len(active) >= NS6:
                    step_all()
                used = {g[1] for g in active}
                j = [x for x in range(NS6) if x not in used][0]
                active.append([comp(i, j), j])
                step_all()
            while active:
                step_all()
        P.barrier()

    def stage_outproj_router(self, st2):
        B, P, I = self, self.P, self.I
        S, NB = self.S, self.NB
        self.NPK = 16 if NB == 1 else 48
        self.affT = B.sb(st2, "affT", [self.NPK, S], F32)
        B.memset("pool", self.affT[:, :], 0.0, w=["affT"])
        with ExitStack() as st:
            W = B.sb(st, "s7_w", [128, 8, D], BF16)
            B.ld(W[:, :, :], I["w_out"].rearrange("(kc p) n -> p kc n", p=128), r=[], w=["s7_w"], eng="pool")
            Wr = B.c_wr
            gB = B.sb(st, "s7_g", [128, D], F32)
            B.ld(gB[:, :], I["norm_ffn_g"].partition_broadcast(128), r=[], w=["s7_g"])
            mx = [B.sb(st, f"s7_mx{i}", [128, 8, 512], BF16) for i in range(2)]
            xt = [B.sb(st, f"s7_x{i}", [128, D], F32) for i in range(2)]
            x1 = [B.sb(st, f"s7_x1{i}", [128, D], F32) for i in range(2)]
            h2 = B.sb(st, "s7_h2", [128, D], F32)
            h2b = [B.sb(st, f"s7_h2b{i}", [128, D], BF16) for i in range(2)]
            h2T = B.sb(st, "s7_h2T", [128, 8, 128], F32)
            junk = B.sb(st, "s7_junk", [128, D], BF16)
            ss = B.sb(st, "s7_ss", [128, 4], F32)
            sm = B.sb(st, "s7_sm", [128, 4], F32)
            ex = B.sb(st, "s7_ex", [128, E], F32)
            aff = B.sb(st, "s7_aff", [128, 48], F32)
            B.memset("pool", aff[:, :], 0.0, w=["s7_aff"])
            po = [B.ps(st, f"s7_po{i}", [128, 512], F32) for i in range(2)]
            pt = [B.ps(st, f"s7_pt{i}", [128, 4, 128], F32) for i in range(2)]
            pl = B.ps(st, "s7_pl", [128, 512], F32)
            pa = B.ps(st, "s7_pa", [128, 512], F32)
            h2s = [h2, B.sb(st, "s7_h2_1", [128, D], F32)]
            h2k = ["s7_h2", "s7_h2_1"]
            sss = [ss, B.sb(st, "s7_ss1", [128, 4], F32)]
            ssk = ["s7_ss", "s7_ss1"]
            NT7 = NB * S // 128

            def ld7(it_):
                j2, tk = it_ % 2, it_ * 128
                if it_ % 4 == 0:
                    g2 = (it_ // 4) % 2
                    B.ld(mx[g2][:, :, :], self.mixT[:, tk:tk + 512].rearrange("(c p) t -> p c t", p=128), r=[], w=[f"s7_mx{g2}"])
                B.ld(xt[j2][:, :], I["x"][tk:tk + 128, :], r=[], w=[f"s7_x{j2}"])

            def phaseA(it):
                tok0 = it * 128
                i2 = it % 2
                if it == 0:
                    ld7(0)
                if it + 1 < NT7:
                    ld7(it + 1)
                for half in range(2):
                    for kc in range(8):
                        g2, o4 = (it // 4) % 2, (it % 4) * 128
                        B.mm(po[half][:, :], mx[g2][:, kc, o4:o4 + 128], W[:, kc, half * 512:(half + 1) * 512], kc == 0, kc == 7,
                             r=[f"s7_mx{g2}", "s7_w"], w=[f"s7_po{half}"])
                    B.tt("dve", x1[i2][:, half * 512:(half + 1) * 512], po[half][:, :], xt[i2][:, half * 512:(half + 1) * 512], OP.add,
                         r=[f"s7_po{half}", f"s7_x{i2}"], w=[f"s7_x1{i2}"])
                B.st_dram(self.x1d[tok0:tok0 + 128, :], x1[i2][:, :], r=[f"s7_x1{i2}"], w=[f"x1d{tok0}"])

            def phaseA2(it):
                tok0 = it * 128
                i2 = it % 2
                B.rmsnorm_rows(x1[i2][:, :], f"s7_x1{i2}", gB[:, :], "s7_g", h2s[i2][:, :], h2k[i2], sss[i2], ssk[i2], junk)
                B.cp("act", h2b[i2][:, :], h2s[i2][:, :], r=[h2k[i2]], w=[f"s7_h2b{i2}"])
                B.st_dram(self.h2d[tok0:tok0 + 128, :], h2b[i2][:, :], r=[f"s7_h2b{i2}"], w=[f"h2d{tok0}"])

            def phaseB(it):
                tok0 = it * 128
                b, t_in = tok0 // S, tok0 % S
                i2 = it % 2
                for q in range(2):
                    for k4 in range(4):
                        kc = q * 4 + k4
                        B.tr(pt[q][:, k4, :], h2s[i2][:, kc * 128:(kc + 1) * 128], B.idf[:, :], r=[h2k[i2], "c_idf"], w=[f"s7_pt{q}"])
                    B.cp("act" if q else "dve", h2T[:, q * 4:(q + 1) * 4, :], pt[q][:, :, :], r=[f"s7_pt{q}"], w=["s7_h2T"])
                for kc in range(8):
                    B.mm(pl[:, 0:E], h2T[:, kc, :], Wr[:, kc, :], kc == 0, kc == 7, r=["s7_h2T", "s7_wr"], w=["s7_pl"])

            def phaseB2(it):
                tok0 = it * 128
                b, t_in = tok0 // S, tok0 % S
                B.P.op("dve", lambda e: e.reduce_max(sm[:, 0:1], pl[:, 0:E], axis=mybir.AxisListType.X), r=["s7_pl"], w=["s7_sm0"])
                B.ts("dve", sm[:, 1:2], sm[:, 0:1], -1.0, OP.mult, r=["s7_sm0"], w=["s7_sm1"])
                B.act(ex[:, :], pl[:, 0:E], AF.Exp, r=["s7_pl", "s7_sm1"], w=["s7_ex", "s7_sm2"], bias=sm[:, 1:2], scale=1.0, accum_out=sm[:, 2:3])
                B.P.op("dve", lambda e: e.reciprocal(sm[:, 3:4], sm[:, 2:3]), r=["s7_sm2"], w=["s7_sm3"])
                B.ts("dve", aff[:, b * 32:b * 32 + E], ex[:, :], sm[:, 3:4], OP.mult, r=["s7_ex", "s7_sm3"], w=["s7_aff"])
                B.tr(pa[0:48, 0:128], aff[:, :], B.idf[:, :], r=["s7_aff", "c_idf"], w=["s7_pa"])
                B.cp("act", self.affT[b * 32:b * 32 + E, t_in:t_in + 128], pa[b * 32:b * 32 + E, 0:128], r=["s7_pa"], w=["affT"])

            for it in range(NT7 + 2):
                if it < NT7:
                    phaseA(it)
                if 0 <= it - 2 < NT7:
                    phaseB2(it - 2)
                if 0 <= it - 1 < NT7:
                    phaseB(it - 1)
                if it < NT7:
                    phaseA2(it)
        P.barrier()

    def stage_topk(self, st2):
        B, P = self, self.P
        S, NB = self.S, self.NB
        cap = 2 * S // E
        SC = min(128, cap)
        nsc = cap // SC
        NPK = self.NPK
        self.cap, self.SC, self.nsc = cap, SC, nsc
        self.idxT = B.sb(st2, "idxT", [128, NB, nsc, E], mybir.dt.int32)
        self.gatT = B.sb(st2, "gatT", [128, NB, nsc, E], F32)
        with ExitStack() as st:
            work = B.sb(st, "s8_work", [NPK, S], F32)
            vals = B.sb(st, "s8_vals", [NPK, cap], F32)
            idxu = B.sb(st, "s8_idxu", [NPK, cap], U32)
            idxf = B.sb(st, "s8_idxf", [NPK, cap], F32)
            pp = B.ps(st, "s8_pp", [128, 512], F32)
            B.cp("dve", work[:, :], self.affT[:, :], r=["affT"], w=["s8_work"])
            for r_ in range(cap // 8):
                sl = slice(r_ * 8, (r_ + 1) * 8)
                B.P.op("dve", lambda e, sl=sl: e.max(vals[:, sl], work[:, :]), r=["s8_work"], w=["s8_vals"])
                B.P.op("dve", lambda e, sl=sl: e.max_index(idxu[:, sl], vals[:, sl], work[:, :]), r=["s8_work", "s8_vals"], w=["s8_idxu"])
                B.P.op("dve", lambda e, sl=sl: e.match_replace(work[:, :], vals[:, sl], work[:, :], -1.0), r=["s8_work", "s8_vals"], w=["s8_work"])
            B.cp("dve", idxf[:, :], idxu[:, :], r=["s8_idxu"], w=["s8_idxf"])
            for b in range(1, NB):
                B.ts("dve", idxf[b * 32:b * 32 + E, :], idxf[b * 32:b * 32 + E, :], float(b * S), OP.add, r=["s8_idxf"], w=["s8_idxf"])
            for sc in range(nsc):
                B.tr(pp[0:SC, 0:NPK], idxf[:, sc * SC:(sc + 1) * SC], B.idf[0:NPK, 0:NPK], r=["s8_idxf", "c_idf"], w=["s8_pp"])
                for b in range(NB):
                    B.cp("dve", self.idxT[0:SC, b, sc, :], pp[0:SC, b * 32:b * 32 + E], r=["s8_pp"], w=["s8_pp", "idxT"])
                B.tr(pp[0:SC, 0:NPK], vals[:, sc * SC:(sc + 1) * SC], B.idf[0:NPK, 0:NPK], r=["s8_vals", "c_idf"], w=["s8_pp"])
                for b in range(NB):
                    B.cp("dve", self.gatT[0:SC, b, sc, :], pp[0:SC, b * 32:b * 32 + E], r=["s8_pp"], w=["s8_pp", "gatT"])
        P.barrier()

    def experts_prefetch(self, st2):
        B, I = self, self.I
        self.Wg = [B.sb(st2, f"s9_wg{i}", [128, 8, FF], BF16) for i in range(2)]
        self.Wu = [B.sb(st2, f"s9_wu{i}", [128, 8, FF], BF16) for i in range(2)]
        self.Wd = [B.sb(st2, f"s9_wd{i}", [128, 8, D], BF16) for i in range(2)]
        for nm, Wt, src in (("wg", self.Wg, "w_e_gate"), ("wu", self.Wu, "w_e_up"), ("wd", self.Wd, "w_e_down")):
            B.ld(Wt[0][:, :, :], I[src][0].rearrange("(kc p) n -> p kc n", p=128), r=[], w=[f"s9_{nm}0"], eng="pool", stream=f"s9pre_{nm}")

    def stage_experts(self):
        B, P, I = self, self.P, self.I
        S, NB = self.S, self.NB
        cap, SC, nsc = self.cap, self.SC, self.nsc
        with ExitStack() as st:
            Wg, Wu, Wd = self.Wg, self.Wu, self.Wd
            XEs = [B.sb(st, f"s9_xe{i}", [128, nsc, D], BF16) for i in range(2)]
            XTs = [B.sb(st, f"s9_xt{i}", [128, 8, cap], BF16) for i in range(2)]
            hTs = [B.sb(st, f"s9_hT{i}", [128, 8, cap], BF16) for i in range(2)]
            sg = B.sb(st, "s9_sg", [128, cap], F32)
            YE = [B.sb(st, f"s9_ye{i}", [128, D], F32) for i in range(2)]
            pst = [B.ps(st, f"s9_pt{i}", [128, 1024], BF16) for i in range(2)]
            pg = [B.ps(st, f"s9_pg{i}", [128, 512], F32) for i in range(2)]
            pu = [B.ps(st, f"s9_pu{i}", [128, 512], F32) for i in range(2)]
            pd = [B.ps(st, f"s9_pd{i}", [128, 512], F32) for i in range(2)]
            nye = 0
            def loadw(e):
                w2 = e % 2
                for nm, Wt, src in (("wg", Wg, "w_e_gate"), ("wu", Wu, "w_e_up"), ("wd", Wd, "w_e_down")):
                    B.ld(Wt[w2][:, :, :], I[src][e].rearrange("(kc p) n -> p kc n", p=128), r=[], w=[f"s9_{nm}{w2}"], eng="pool")
            def gather(it):
                e_, b_ = it // NB, it % NB
                j2 = it % 2
                for sc in range(nsc):
                    B.P.dma("pool", lambda eng, sc=sc, b_=b_, e_=e_, j2=j2: eng.indirect_dma_start(
                        out=XEs[j2][0:SC, sc, :], out_offset=None, in_=self.h2d[:, :],
                        in_offset=bass.IndirectOffsetOnAxis(ap=self.idxT[0:SC, b_, sc, e_:e_ + 1], axis=0)),
                        r=["idxT"], w=[f"s9_xe{j2}"], stream=f"s9_xe{j2}")
            for e in range(E):
                w2 = e % 2
                if e + 1 < E:
                    loadw(e + 1)
                for b in range(NB):
                    it = e * NB + b
                    i2 = it % 2
                    XE, XT, hT = XEs[i2], XTs[i2], hTs[i2]
                    kxe, kxt, khT = f"s9_xe{i2}", f"s9_xt{i2}", f"s9_hT{i2}"
                    if it == 0:
                        gather(0)
                    if it + 1 < E * NB:
                        gather(it + 1)
                    for kc in range(8):
                        pt_ = pst[kc % 2]; ptk = f"s9_pt{kc % 2}"
                        for sc in range(nsc):
                            B.tr(pt_[:, sc * SC:(sc + 1) * SC], XE[0:SC, sc, kc * 128:(kc + 1) * 128], B.idb[0:SC, 0:SC], r=[kxe, "c_idb"], w=[ptk])
                        B.cp("act" if kc % 2 else "dve", XT[:, kc, :], pt_[:, 0:cap], r=[ptk], w=[kxt])
                    for fc in range(8):
                        f2 = fc % 2
                        fs = slice(fc * 128, (fc + 1) * 128)
                        for kc in range(8):
                            B.mm(pg[f2][:, 0:cap], Wg[w2][:, kc, fs], XT[:, kc, :], kc == 0, kc == 7, r=[f"s9_wg{w2}", kxt], w=[f"s9_pg{f2}"])
                        for kc in range(8):
                            B.mm(pu[f2][:, 0:cap], Wu[w2][:, kc, fs], XT[:, kc, :], kc == 0, kc == 7, r=[f"s9_wu{w2}", kxt], w=[f"s9_pu{f2}"])
                        B.act(sg[:, :], pg[f2][:, 0:cap], AF.Silu, r=[f"s9_pg{f2}"], w=["s9_sg"])
                        B.tt("dve", hT[:, fc, :], pu[f2][:, 0:cap], sg[:, :], OP.mult, r=[f"s9_pu{f2}", "s9_sg"], w=[khT])
                    for sc in range(nsc):
                        ye = YE[nye % 2]; yk = f"s9_ye{nye % 2}"; nye += 1
                        for dh in range(2):
                            for fc in range(8):
                                B.mm(pd[dh][0:SC, :], hT[:, fc, sc * SC:(sc + 1) * SC], Wd[w2][:, fc, dh * 512:(dh + 1) * 512], fc == 0, fc == 7,
                                     r=[khT, f"s9_wd{w2}"], w=[f"s9_pd{dh}"])
                            B.ts("dve", ye[0:SC, dh * 512:(dh + 1) * 512], pd[dh][0:SC, :], self.gatT[0:SC, b, sc, e:e + 1], OP.mult,
                                 r=[f"s9_pd{dh}", "gatT"], w=[yk])
                        B.P.dma("pool", lambda eng, ye=ye, sc=sc, b=b, e=e: eng.indirect_dma_start(
                            out=self.x1d[:, :], out_offset=bass.IndirectOffsetOnAxis(ap=self.idxT[0:SC, b, sc, e:e + 1], axis=0),
                            in_=ye[0:SC, :], in_offset=None, compute_op=OP.add),
                            r=[yk, "idxT"], w=["x1d_acc"], stream="st_" + yk)
        P.barrier()

    def stage_final(self):
        B, P, I = self, self.P, self.I
        with ExitStack() as st:
            gB = B.sb(st, "s10_g", [128, D], F32)
            B.ld(gB[:, :], I["final_norm_g"].partition_broadcast(128), r=[], w=["s10_g"])
            xt = [B.sb(st, f"s10_x{i}", [128, D], F32) for i in range(2)]
            ot = [B.sb(st, f"s10_o{i}", [128, D], F32) for i in range(2)]
            tm10 = [B.sb(st, f"s10_t{i}", [128, D], F32) for i in range(2)]
            junk = B.sb(st, "s10_junk", [128, D], BF16)
            ss = [B.sb(st, f"s10_ss{i}", [128, 4], F32) for i in range(2)]
            for it in range(self.T // 128):
                i2 = it % 2
                if it == 0:
                    B.ld(xt[0][:, :], self.x1d[0:128, :], r=[], w=["s10_x0"])
                if it + 1 < self.T // 128:
                    B.ld(xt[1 - i2][:, :], self.x1d[(it + 1) * 128:(it + 2) * 128, :], r=[], w=[f"s10_x{1 - i2}"])
                B.rmsnorm_rows(xt[i2][:, :], f"s10_x{i2}", gB[:, :], "s10_g", ot[i2][:, :], f"s10_o{i2}", ss[i2], f"s10_ss{i2}", junk,
                               tmp=tm10[i2][:, :], tmpk=f"s10_t{i2}")
                B.st_dram(self.out[it * 128:(it + 1) * 128, :], ot[i2][:, :], r=[f"s10_o{i2}"], w=[f"out{it}"])
        P.barrier()


def build(S, NB, debug=False, upto=99):
    B = Builder(S, NB, debug)
    B.declare_io()
    with B.stack:
        cst = B.stack.enter_context(ExitStack())
        B.consts(cst)
        B.P.barrier()
        B.stage_inproj()
        if upto >= 2:
            B.stage_attn()
        if upto >= 3:
            B.stage_conv()
        if upto >= 4:
            B.stage_rwkv_prep()
        if upto >= 5:
            B.stage_rwkv_scan()
        if upto >= 6:
            B.stage_rwkv_post()
        if upto >= 7:
            st2 = B.stack.enter_context(ExitStack())
            B.stage_outproj_router(st2)
        if upto >= 9:
            B.experts_prefetch(st2)
        if upto >= 8:
            B.stage_topk(st2)
        if upto >= 9:
            B.stage_experts()
            B.stage_final()
        B.P.emit()
    return B


def prep(inp, b0, NB):
    m = {}
    for k, v in inp.items():
        v = np.asarray(v)
        if k == "x":
            m[k] = np.ascontiguousarray(v[b0:b0 + NB].reshape(NB * v.shape[1], D))
        elif k == "mem":
            m[k] = np.ascontiguousarray(v[b0:b0 + NB].reshape(NB * NMEM, D))
        elif k == "final_norm_g":
            m[k] = np.ascontiguousarray(v)
        else:
            a = v[0]
            if k in ("decay_up", "iclr_up"):
                a = a.reshape(128, 384)
            if k == "r_k":
                a = a.reshape(384)
            m[k] = np.ascontiguousarray(a)
    return m


_CACHE = {}


def kernel(**inputs):
    S, NB, NC = 4096, 2, 8
    if "B" not in _CACHE:
        _CACHE["B"] = build(S, NB, debug=False)
    B = _CACHE["B"]
    in_maps = [prep(inputs, c * NB, NB) for c in range(NC)]
    res = run_bass_kernel_spmd(B.nc, in_maps, core_ids=list(range(NC)))
    outs = [np.asarray(r["out"], dtype=np.float32).reshape(NB, S, D) for r in res.results]
    return np.concatenate(outs, axis=0)
```

```python
import numpy as np
from contextlib import ExitStack
import concourse.bass as bass
import concourse.mybir as mybir
from concourse.bass_utils import run_bass_kernel_spmd

F32 = mybir.dt.float32
BF16 = mybir.dt.bfloat16
U32 = mybir.dt.uint32
AF = mybir.ActivationFunctionType
OP = mybir.AluOpType

D = 1024
NMEM = 256
INP = 2560
E = 16
FF = 1024
SEM_LIMIT = 30000


class Ctr:
    def __init__(self, prog, name, step):
        self.prog, self.name, self.step = prog, name, step
        self.sem = None
        self.val = 0
        self.n = 0

    def next(self):
        if self.sem is None and self.step == 16 and self.prog.free_dma_sems and not getattr(self, "sw", False):
            self.sem, self.val = self.prog.free_dma_sems.pop()
        if self.sem is None or self.val + self.step > SEM_LIMIT:
            self.sem = self.prog.new_sem(f"s{self.prog.nsem}")
            if self.name == "pe":
                self.prog.pe_sems.add(id(self.sem))
            self.n += 1
            self.val = 0
        self.val += self.step
        return (self.sem, self.val)


class Prog:
    ENG = ("pe", "act", "dve", "pool", "sp")

    def __init__(self, nc, stack):
        self.nc, self.stack = nc, stack
        self.ops = {e: [] for e in self.ENG}
        self.ctr = {e: Ctr(self, e, 1) for e in self.ENG}
        self.dctr = {}
        self.lastw = {}
        self.reads = {}
        self.known = {e: {} for e in self.ENG}
        self.nsem = 0
        self.all_events = {}
        self.pe_sems = set()
        self.free_dma_sems = []

    def new_sem(self, name):
        self.nsem += 1
        return self.stack.enter_context(self.nc.semaphore(name))

    def _deps(self, eng, r, w, force=False):
        deps = []
        for k in r:
            ev = self.lastw.get(k)
            if ev is not None:
                deps.append(ev)
        for k in w:
            ev = self.lastw.get(k)
            if ev is not None:
                deps.append(ev)
            deps.extend(self.reads.get(k, {}).items())
        waits = []
        kn = self.known[eng]
        for sem, val in deps:
            if eng == "pe" and id(sem) in self.pe_sems and not force:
                continue
            if kn.get(sem, 0) >= val:
                continue
            kn[sem] = val
            waits.append((sem, val))
        best = {}
        for sem, val in waits:
            best[sem] = max(best.get(sem, 0), val)
        return list(best.items())

    def _commit(self, ev, r, w):
        sem, val = ev
        self.all_events[sem] = val
        for k in r:
            d = self.reads.setdefault(k, {})
            d[sem] = max(d.get(sem, 0), val)
        for k in w:
            self.lastw[k] = ev
            self.reads[k] = {}

    def op(self, eng, fn, r=(), w=(), force=False):
        waits = self._deps(eng, r, w, force)
        ev = self.ctr[eng].next()
        self.ops[eng].append((waits, fn, ev, 1))
        self._commit(ev, r, w)

    def dma(self, eng, fn, r=(), w=(), stream=None):
        waits = self._deps(eng, r, w)
        if stream is None:
            stream = "d_" + str((tuple(w) or tuple(r))[0])
        c = self.dctr.get(stream)
        if c is None:
            c = self.dctr[stream] = Ctr(self, stream, 16)
            c.sw = (eng == "pool")
        ev = c.next()
        self.ops[eng].append((waits, fn, ev, 16))
        self._commit(ev, r, w)

    def barrier(self):
        for eng in self.ENG:
            waits = []
            kn = self.known[eng]
            for sem, val in self.all_events.items():
                if kn.get(sem, 0) >= val:
                    continue
                kn[sem] = val
                waits.append((sem, val))
            if waits:
                self.ops[eng].append((waits, None, None, 0))
        self.lastw.clear()
        self.reads.clear()
        for c in self.dctr.values():
            if c.sem is not None and not getattr(c, "sw", False):
                self.free_dma_sems.append((c.sem, c.val))
        self.dctr.clear()

    def emit(self):
        nc = self.nc
        with nc.Block() as block:
            def mk(name):
                def body(engobj):
                    for waits, fn, ev, step in self.ops[name]:
                        for sem, val in waits:
                            engobj.wait_ge(sem, val)
                        if fn is not None:
                            inst = fn(engobj)
                            inst.then_inc(ev[0], step)
                return body
            block.tensor(mk("pe"))
            block.scalar(mk("act"))
            block.vector(mk("dve"))
            block.gpsimd(mk("pool"))
            block.sync(mk("sp"))


class Builder:
    def __init__(self, S, NB, debug=False):
        self.S, self.NB, self.T = S, NB, S * NB
        self.debug = debug
        self.nc = bass.Bass("TRN2", target_bir_lowering=False)
        self.stack = ExitStack()
        self.P = Prog(self.nc, self.stack)
        self.uid = 0
        self.dbg_outs = []

    def dram_in(self, name, shape, dt=F32):
        return self.nc.dram_tensor(name, list(shape), dt, kind="ExternalInput").ap()

    def dram_out(self, name, shape, dt=F32):
        return self.nc.dram_tensor(name, list(shape), dt, kind="ExternalOutput").ap()

    def scratch(self, name, shape, dt=F32, dbg=False):
        if self.debug and dbg:
            self.dbg_outs.append(name)
            return self.nc.dram_tensor(name, list(shape), dt, kind="ExternalOutput").ap()
        return self.nc.dram_tensor(name, list(shape), dt, kind="Internal").ap()

    def sb(self, st, name, shape, dt=F32):
        return st.enter_context(self.nc.sbuf_tensor(name, list(shape), dt))

    def ps(self, st, name, shape, dt=F32):
        return st.enter_context(self.nc.psum_tensor(name, list(shape), dt))

    def mm(self, out, lhsT, rhs, start, stop, r, w, force=False):
        self.P.op("pe", lambda e: e.matmul(out, lhsT, rhs, start=start, stop=stop), r=r, w=w, force=force)

    def tr(self, out, in_, ident, r, w):
        self.P.op("pe", lambda e: e.transpose(out, in_, ident), r=r, w=w)

    def act(self, out, in_, func, r, w, bias=None, scale=None, accum_out=None):
        kw = {}
        if bias is not None:
            kw["bias"] = bias
        if scale is not None:
            kw["scale"] = scale
        if accum_out is not None:
            kw["accum_out"] = accum_out
        self.P.op("act", lambda e: e.activation(out, in_, func, **kw), r=r, w=w)

    def tt(self, eng, out, in0, in1, op, r, w):
        self.P.op(eng, lambda e: e.tensor_tensor(out, in0, in1, op), r=r, w=w)

    def ts(self, eng, out, in0, s1, op0, r, w, s2=None, op1=None):
        if op1 is None:
            self.P.op(eng, lambda e: e.tensor_scalar(out, in0, s1, None, op0), r=r, w=w)
        else:
            self.P.op(eng, lambda e: e.tensor_scalar(out, in0, s1, s2, op0, op1), r=r, w=w)

    def stt(self, out, in0, scalar, in1, op0, op1, r, w):
        self.P.op("dve", lambda e: e.scalar_tensor_tensor(out, in0, scalar, in1, op0, op1), r=r, w=w)

    def cp(self, eng, out, in_, r, w):
        if eng == "act":
            self.P.op("act", lambda e: e.copy(out, in_), r=r, w=w)
        else:
            self.P.op(eng, lambda e: e.tensor_copy(out, in_), r=r, w=w)

    def memset(self, eng, out, val, w):
        self.P.op(eng, lambda e: e.memset(out, val), r=(), w=w)

    def ld(self, out, in_, r, w, eng="sp", stream=None, nc_ok=False):
        kw = {"allow_slow_non_contiguous": True} if nc_ok else {}
        self.P.dma(eng, lambda e: e.dma_start(out=out, in_=in_, **kw), r=r, w=w, stream=stream)

    def st_dram(self, out, in_, r, w, eng="sp", stream=None):
        if stream is None:
            stream = "st_" + str(r[0])
        self.P.dma(eng, lambda e: e.dma_start(out=out, in_=in_), r=r, w=w, stream=stream)

    def declare_io(self):
        T, NB = self.T, self.NB
        I = {}
        I["x"] = self.dram_in("x", [T, D])
        I["mem"] = self.dram_in("mem", [NB * NMEM, D])
        for nm, shp in [("norm_mix_g", [D]), ("norm_mem_g", [D]), ("w_in", [D, INP]), ("w_mk", [D, 256]),
                        ("w_mv", [D, 256]), ("conv_w", [31, 384]), ("conv_b", [384]), ("conv_ln_g", [384]),
                        ("conv_ln_b", [384]), ("shift_mu", [2, 1536]), ("decay_w0", [2, 384]),
                        ("decay_up", [128, 384]), ("iclr_a0", [2, 384]), ("iclr_up", [128, 384]),
                        ("gate_up", [128, 384]), ("k_k", [384]), ("k_a", [384]), ("r_k", [384]),
                        ("lnx_g", [384]), ("lnx_b", [384]), ("w_out", [D, D]), ("norm_ffn_g", [D]),
                        ("w_router", [D, E]), ("w_e_gate", [E, D, FF]), ("w_e_up", [E, D, FF]),
                        ("w_e_down", [E, FF, D]), ("final_norm_g", [D])]:
            I[nm] = self.dram_in(nm, shp)
        self.I = I
        self.out = self.dram_out("out", [T, D])
        self.pT = self.scratch("pT", [INP, T], F32, dbg=True)
        self.mixT = self.scratch("mixT", [D, T], BF16, dbg=True)
        self.FM = [self.scratch(f"FM{d}", [4, 384, T], F32, dbg=True) for d in range(2)]
        self.TM = [self.scratch(f"TM{d}", [T, 3, 384], F32, dbg=True) for d in range(2)]
        self.TMV = self.scratch("TMV", [T, 384], F32, dbg=True)
        self.PLd = [self.scratch(f"PL{d}", [384, T // 64], F32, dbg=True) for d in range(2)]
        self.gT = self.scratch("gT", [384, T], F32, dbg=True)
        self.bonT = self.scratch("bonT", [384, T], F32, dbg=True)
        self.YT = [self.scratch(f"YT{d}", [384, T], F32, dbg=True) for d in range(2)]
        self.x1d = self.scratch("x1d", [T, D], F32, dbg=True)
        self.h2d = self.scratch("h2d", [T, D], BF16, dbg=True)

    def consts(self, st):
        B = self
        io = B.sb(st, "c_iota", [128, 128], mybir.dt.int32)
        B.P.op("pool", lambda e: e.iota(io[:, :], [[1, 128]], base=0, channel_multiplier=-1), w=["c_iota"])
        B.idf = B.sb(st, "c_idf", [128, 128], F32)
        B.idb = B.sb(st, "c_idb", [128, 128], BF16)
        B.dif = B.sb(st, "c_dif", [128, 128], F32)
        B.cp("dve", B.dif[:, :], io[:, :], r=["c_iota"], w=["c_dif"])
        B.ts("dve", B.idf[:, :], B.dif[:, :], 0.0, OP.is_equal, r=["c_dif"], w=["c_idf"])
        B.cp("dve", B.idb[:, :], B.idf[:, :], r=["c_idf"], w=["c_idb"])
        B.blk = B.sb(st, "c_blk", [128, 128], F32)
        B.memset("dve", B.blk[:, :], 0.0, w=["c_blk"])
        B.memset("dve", B.blk[0:64, 0:64], 1.0, w=["c_blk"])
        B.memset("dve", B.blk[64:128, 64:128], 1.0, w=["c_blk"])
        B.epsc = B.sb(st, "c_eps", [128, 4], F32)
        for i, v in enumerate([1e-6, 1e-5, 64e-5, 1e-12]):
            B.memset("dve", B.epsc[:, i:i + 1], v, w=["c_eps"])
        B.eps6 = B.epsc[:, 0:1]
        I = B.I
        B.c_cwT = B.sb(st, "c_cwT", [128, 3, 31], F32)
        for c in range(3):
            B.ld(B.c_cwT[:, c, :], I["conv_w"][:, c * 128:(c + 1) * 128].rearrange("k p -> p k"), r=[], w=["c_cwT"], nc_ok=True, eng="pool", stream="c_cwT")
        B.c_vec3 = B.sb(st, "c_vec3", [128, 3, 3], F32)
        for j, nm in enumerate(["conv_b", "conv_ln_g", "conv_ln_b"]):
            B.ld(B.c_vec3[:, j, :], I[nm].rearrange("(c p) -> p c", p=128), r=[], w=["c_vec3"], nc_ok=True, eng="pool", stream="c_vec3")
        B.c_mu = B.sb(st, "c_mu", [128, 3, 12], F32)
        for j in range(2):
            B.ld(B.c_mu[:, 1 + j, :], I["shift_mu"][j, :].rearrange("(c p) -> p c", p=128), r=[], w=["c_mu"], nc_ok=True, eng="pool", stream="c_mu")

        def vecload(name, ap, shape):
            t = B.sb(st, name, shape, F32)
            B.ld(t[tuple(slice(None) for _ in shape)], ap, r=[], w=[name], nc_ok=True, eng="pool", stream=name)
            return t
        B.c_w0 = vecload("c_w0", I["decay_w0"].rearrange("d (c p) -> p d c", p=128), [128, 2, 3])
        B.c_a0 = vecload("c_a0", I["iclr_a0"].rearrange("d (c p) -> p d c", p=128), [128, 2, 3])
        B.c_kkv = vecload("c_kkv", I["k_k"].rearrange("(c p) -> p c", p=128), [128, 3])
        B.c_ka = vecload("c_ka", I["k_a"].rearrange("(c p) -> p c", p=128), [128, 3])
        B.c_rk = vecload("c_rk", I["r_k"].rearrange("(c p) -> p c", p=128), [128, 3])

    def stage_inproj(self):
        B, P, I = self, self.P, self.I
        T = self.T
        with ExitStack() as st:
            W = B.sb(st, "s1_w", [128, 8, INP], BF16)
            wv = I["w_in"].rearrange("(kc p) n -> p kc n", p=128)
            for hh in range(2):
                B.ld(W[:, :, hh * 1280:(hh + 1) * 1280], wv[:, :, hh * 1280:(hh + 1) * 1280], r=[], w=["s1_w"],
                     eng="pool", stream="s1_w")
            gB = B.sb(st, "s1_g", [128, D], F32)
            B.ld(gB[:, :], I["norm_mix_g"].partition_broadcast(128), r=[], w=["s1_g"])
            xin = [B.sb(st, f"s1_x{i}", [128, D], F32) for i in range(2)]
            junk = B.sb(st, "s1_junk", [128, D], BF16)
            hb = [B.sb(st, f"s1_hb{i}", [128, D], BF16) for i in range(2)]
            hT = [B.sb(st, f"s1_hT{i}", [128, 8, 512], BF16) for i in range(2)]
            ss = [B.sb(st, f"s1_ss{i}", [128, 4], F32) for i in range(2)]
            og = [B.sb(st, f"s1_o{i}", [128, 5, 512], F32) for i in range(3)]
            pst = [B.ps(st, f"s1_pt{i}", [128, 8, 128], BF16) for i in range(2)]
            pm = [B.ps(st, f"s1_pm{i}", [128, 512], F32) for i in range(4)]
            nt = T // 512
            n128 = 0
            nmm = 0
            ngrp = 0
            for t5 in range(nt):
                hk = f"s1_hT{t5 % 2}"
                hTt = hT[t5 % 2]
                for sub in range(4):
                    i2 = n128 % 2
                    if n128 == 0:
                        B.ld(xin[0][:, :], I["x"][0:128, :], r=[], w=["s1_x0"])
                    n128 += 1
                    tok0 = t5 * 512 + sub * 128
                    if tok0 + 128 < T:
                        B.ld(xin[1 - i2][:, :], I["x"][tok0 + 128:tok0 + 256, :], r=[], w=[f"s1_x{1 - i2}"])
                    B.act(junk[:, :], xin[i2][:, :], AF.Square, r=[f"s1_x{i2}"], w=["s1_junk", f"s1_ss{i2}a"],
                          accum_out=ss[i2][:, 0:1])
                    B.act(ss[i2][:, 1:2], ss[i2][:, 0:1], AF.Sqrt, r=[f"s1_ss{i2}a"], w=[f"s1_ss{i2}b"],
                          bias=B.eps6, scale=1.0 / D)
                    B.P.op("dve", lambda e, o=ss[i2][:, 2:3], i=ss[i2][:, 1:2]: e.reciprocal(o, i),
                           r=[f"s1_ss{i2}b"], w=[f"s1_ss{i2}c"])
                    B.stt(hb[i2][:, :], xin[i2][:, :], ss[i2][:, 2:3], gB[:, :], OP.mult, OP.mult,
                          r=[f"s1_x{i2}", f"s1_ss{i2}c", "s1_g"], w=[f"s1_hb{i2}"])
                    for kc in range(8):
                        B.tr(pst[i2][:, kc, :], hb[i2][:, kc * 128:(kc + 1) * 128], B.idb[:, :],
                             r=[f"s1_hb{i2}", "c_idb"], w=[f"s1_pt{i2}"])
                    B.cp("act" if sub % 2 else "dve", hTt[:, :, sub * 128:(sub + 1) * 128], pst[i2][:, :, :],
                         r=[f"s1_pt{i2}"], w=[hk])
                for cg in range(4):
                    o = og[ngrp % 3]
                    ok = f"s1_o{ngrp % 3}"
                    ngrp += 1
                    for c5 in range(5):
                        c = cg * 5 + c5
                        pmi = nmm % 4
                        nmm += 1
                        for kc in range(8):
                            B.mm(pm[pmi][:, :], W[:, kc, c * 128:(c + 1) * 128], hTt[:, kc, :], kc == 0, kc == 7,
                                 r=["s1_w", hk], w=[f"s1_pm{pmi}"])
                        B.cp("act" if c % 2 else "dve", o[:, c5, :], pm[pmi][:, :], r=[f"s1_pm{pmi}"], w=[ok])
                    dst = self.pT[cg * 640:(cg + 1) * 640, t5 * 512:(t5 + 1) * 512].rearrange("(c p) t -> p c t", p=128)
                    B.st_dram(dst, o[:, :, :], r=[ok], w=[f"pT_{t5}_{cg}"], stream="st_" + ok)
        P.barrier()

    def rmsnorm_rows(self, xt, xk, gB, gk, out, outk, ss, ssk, junk, tmp=None, tmpk=None):
        B = self
        B.act(junk[:, :], xt, AF.Square, r=[xk], w=["junk", ssk + "a"], accum_out=ss[:, 0:1])
        B.act(ss[:, 1:2], ss[:, 0:1], AF.Sqrt, r=[ssk + "a"], w=[ssk + "b"], bias=B.eps6, scale=1.0 / D)
        B.P.op("dve", lambda e: e.reciprocal(ss[:, 2:3], ss[:, 1:2]), r=[ssk + "b"], w=[ssk + "c"])
        if tmp is None:
            B.stt(out, xt, ss[:, 2:3], gB, OP.mult, OP.mult, r=[xk, ssk + "c", gk], w=[outk])
        else:
            B.act(tmp, xt, AF.Copy, r=[xk, ssk + "c"], w=[tmpk], scale=ss[:, 2:3])
            B.tt("dve", out, tmp, gB, OP.mult, r=[tmpk, gk], w=[outk])

    def stage_attn(self):
        B, P, I = self, self.P, self.I
        S, NB = self.S, self.NB
        with ExitStack() as st:
            Wk = B.sb(st, "s2_wk", [128, 8, 256], BF16)
            Wv = B.sb(st, "s2_wv", [128, 8, 256], BF16)
            B.ld(Wk[:, :, :], I["w_mk"].rearrange("(kc p) n -> p kc n", p=128), r=[], w=["s2_wk"], eng="pool")
            B.ld(Wv[:, :, :], I["w_mv"].rearrange("(kc p) n -> p kc n", p=128), r=[], w=["s2_wv"], eng="pool")
            gB = B.sb(st, "s2_g", [128, D], F32)
            B.ld(gB[:, :], I["norm_mem_g"].partition_broadcast(128), r=[], w=["s2_g"])
            ON = B.sb(st, "s2_on", [128, 2, 128], BF16)
            B.memset("dve", ON[:, :, :], 0.0, w=["s2_on"])
            B.memset("dve", ON[:, 0, 0:64], 1.0, w=["s2_on"])
            B.memset("dve", ON[:, 1, 64:128], 1.0, w=["s2_on"])
            mt = B.sb(st, "s2_m", [128, D], F32)
            junk = B.sb(st, "s2_junk", [128, D], BF16)
            mb = B.sb(st, "s2_mb", [128, D], BF16)
            ss = B.sb(st, "s2_ss", [128, 4], F32)
            memT = B.sb(st, "s2_memT", [128, 8, 256], BF16)
            KT = B.sb(st, "s2_KT", [128, 2, 256], BF16)
            VA = B.sb(st, "s2_VA", [128, 2, 4, 128], BF16)
            qf = [B.sb(st, f"s2_qf{i}", [128, 2, 512], F32) for i in range(2)]
            qbs = [B.sb(st, f"s2_qb{i}", [128, 2, 512], BF16) for i in range(2)]
            PTs = [[B.sb(st, f"s2_PT{i}_{h}", [128, 2, 512], BF16) for h in range(4)] for i in range(2)]
            cnt2 = [0]
            rd = B.sb(st, "s2_rd", [128, 512], F32)
            ob = [B.sb(st, f"s2_ob{i}", [128, 2, 512], BF16) for i in range(2)]
            pst = B.ps(st, "s2_pt", [128, 8, 128], BF16)
            pa = [B.ps(st, f"s2_pa{i}", [128, 512], F32) for i in range(4)]
            pn = B.ps(st, "s2_pn", [128, 512], F32)
            pd = B.ps(st, "s2_pd", [128, 512], F32)
            npa = 0
            nq = 0
            for b in range(NB):
                for mc in range(2):
                    B.ld(mt[:, :], I["mem"][b * 256 + mc * 128: b * 256 + (mc + 1) * 128, :], r=[], w=["s2_m"])
                    B.rmsnorm_rows(mt[:, :], "s2_m", gB[:, :], "s2_g", mb[:, :], "s2_mb", ss, "s2_ss", junk)
                    for kc in range(8):
                        B.tr(pst[:, kc, :], mb[:, kc * 128:(kc + 1) * 128], B.idb[:, :], r=["s2_mb", "c_idb"], w=["s2_pt"])
                    B.cp("dve", memT[:, :, mc * 128:(mc + 1) * 128], pst[:, :, :], r=["s2_pt"], w=["s2_memT"])
                for hc in range(2):
                    p = pa[npa % 4]; pk = f"s2_pa{npa % 4}"; npa += 1
                    for kc in range(8):
                        B.mm(p[:, 0:256], Wk[:, kc, hc * 128:(hc + 1) * 128], memT[:, kc, :], kc == 0, kc == 7,
                             r=["s2_wk", "s2_memT"], w=[pk])
                    B.cp("act", KT[:, hc, :], p[:, 0:256], r=[pk], w=["s2_KT"])
                B.memset("pool", VA[:, :, :, :], 0.0, w=["s2_VA"])
                for mc in range(2):
                    p = pa[npa % 4]; pk = f"s2_pa{npa % 4}"; npa += 1
                    for kc in range(8):
                        B.mm(p[:, 0:256], memT[:, kc, mc * 128:(mc + 1) * 128], Wv[:, kc, :], kc == 0, kc == 7,
                             r=["s2_wv", "s2_memT"], w=[pk])
                    for h in range(4):
                        B.cp("dve", VA[:, mc, h, (h % 2) * 64:(h % 2) * 64 + 64], p[:, h * 64:(h + 1) * 64], r=[pk], w=["s2_VA"])
                NT2 = S // 512

                def scores(t5):
                    tok0 = b * S + t5 * 512
                    j = t5 % 2
                    q, qk = qf[j], f"s2_qf{j}"
                    B.ld(q[:, :, :], self.pT[0:256, tok0:tok0 + 512].rearrange("(c p) t -> p c t", p=128), r=[], w=[qk])
                    B.cp("dve", qbs[j][:, :, :], q[:, :, :], r=[qk], w=[f"s2_qb{j}"])
                    for h in range(4):
                        hc, base = h // 2, (h % 2) * 64
                        for mc in range(2):
                            cnt2[0] += 1
                            p = pa[cnt2[0] % 4]; pk = f"s2_pa{cnt2[0] % 4}"
                            B.mm(p[:, :], KT[base:base + 64, hc, mc * 128:(mc + 1) * 128], qbs[j][base:base + 64, hc, :], True, True,
                                 r=["s2_KT", f"s2_qb{j}"], w=[pk])
                            B.act(PTs[j][h][:, mc, :], p[:, :], AF.Exp, r=[pk], w=[f"s2_PT{j}_{h}"], scale=0.125)

                def pv(t5):
                    tok0 = b * S + t5 * 512
                    j = t5 % 2
                    o, okk = ob[j], f"s2_ob{j}"
                    for pr in range(2):
                        i = 0
                        for h in (2 * pr, 2 * pr + 1):
                            for mc in range(2):
                                B.mm(pn[:, :], VA[:, mc, h, :], PTs[j][h][:, mc, :], i == 0, i == 3, r=["s2_VA", f"s2_PT{j}_{h}"], w=["s2_pn"])
                                i += 1
                        i = 0
                        for h in (2 * pr, 2 * pr + 1):
                            for mc in range(2):
                                B.mm(pd[:, :], ON[:, h % 2, :], PTs[j][h][:, mc, :], i == 0, i == 3, r=["s2_on", f"s2_PT{j}_{h}"], w=["s2_pd"])
                                i += 1
                        B.P.op("dve", lambda e: e.reciprocal(rd[:, :], pd[:, :]), r=["s2_pd"], w=["s2_rd"])
                        B.tt("dve", o[:, pr, :], pn[:, :], rd[:, :], OP.mult, r=["s2_pn", "s2_rd"], w=[okk])
                    B.st_dram(self.mixT[0:256, tok0:tok0 + 512].rearrange("(c p) t -> p c t", p=128), o[:, :, :], r=[okk], w=[f"mixA_{tok0}"],
                              stream="st_" + okk, eng="pool")

                scores(0)
                for t5 in range(NT2):
                    if t5 + 1 < NT2:
                        scores(t5 + 1)
                    pv(t5)
        P.barrier()

    def stage_conv(self):
        B, P, I = self, self.P, self.I
        S, NB = self.S, self.NB
        with ExitStack() as st:
            cwT = B.c_cwT
            vec = B.c_vec3
            DG = B.sb(st, "s3_dg", [128, 3, 31, 128], BF16)
            for c in range(3):
                for k in range(31):
                    B.ts("dve", DG[:, c, k, :], B.idf[:, :], cwT[:, c, k:k + 1], OP.mult, r=["c_idf", "s3_cwT"], w=["s3_dg"])
            ones = B.sb(st, "s3_ones", [128, 128], F32)
            B.memset("dve", ones[:, :], 1.0 / 384.0, w=["s3_ones"])
            U = B.sb(st, "s3_U", [128, 3, S + 30], BF16)
            vf = [B.sb(st, f"s3_vf{i}", [128, 6, 512], F32) for i in range(2)]
            sg = B.sb(st, "s3_sg", [128, 3, 512], F32)
            Cf = B.sb(st, "s3_Cf", [128, 3, 512], F32)
            Cs = B.sb(st, "s3_Cs", [128, 3, 512], F32)
            mS = B.sb(st, "s3_mS", [128, 512], F32)
            m2 = B.sb(st, "s3_m2", [128, 512], F32)
            rs = B.sb(st, "s3_rs", [128, 512], F32)
            y = B.sb(st, "s3_y", [128, 512], F32)
            z = B.sb(st, "s3_z", [128, 512], F32)
            ob = [B.sb(st, f"s3_ob{i}", [128, 3, 512], BF16) for i in range(2)]
            pc = [B.ps(st, f"s3_pc{i}", [128, 512], F32) for i in range(3)]
            pmean = B.ps(st, "s3_pmean", [128, 512], F32)
            pmsq = B.ps(st, "s3_pmsq", [128, 512], F32)
            nv = [0]
            NT3 = S // 512
            B.memset("pool", U[:, :, 0:15], 0.0, w=["s3_Uh"])
            B.memset("pool", U[:, :, S + 15:S + 30], 0.0, w=["s3_Uh"])

            def uload(b, t5):
                tok0 = b * S + t5 * 512
                v = vf[t5 % 2]; vk = f"s3_vf{t5 % 2}"
                B.ld(v[:, :, :], self.pT[256:1024, tok0:tok0 + 512].rearrange("(c p) t -> p c t", p=128), r=[], w=[vk])

            def ubuild(b, t5):
                v = vf[t5 % 2]; vk = f"s3_vf{t5 % 2}"
                B.act(sg[:, :, :], v[:, 3:6, :], AF.Sigmoid, r=[vk], w=["s3_sg"])
                B.tt("dve", U[:, :, 15 + t5 * 512: 15 + (t5 + 1) * 512], v[:, 0:3, :], sg[:, :, :], OP.mult, r=[vk, "s3_sg"], w=[f"s3_U{t5}"])

            items3 = [(b, t5) for b in range(NB) for t5 in range(NT3)]
            for ii, (b, t5) in enumerate(items3):
                if t5 == 0:
                    uload(b, 0)
                    if NT3 > 1:
                        uload(b, 1)
                    ubuild(b, 0)
                    if NT3 > 2:
                        uload(b, 2)
                    if NT3 > 1:
                        ubuild(b, 1)
                if t5 + 3 < NT3:
                    uload(b, t5 + 3)
                if t5 + 2 < NT3:
                    ubuild(b, t5 + 2)
                if True:
                    tok0 = b * S + t5 * 512
                    o = ob[t5 % 2]; okk = f"s3_ob{t5 % 2}"
                    ukeys = [f"s3_U{j}" for j in (t5 - 1, t5, t5 + 1) if 0 <= j < NT3] + ["s3_Uh"]
                    for c in range(3):
                        for k in range(31):
                            B.mm(pc[c][:, :], DG[:, c, k, :], U[:, c, t5 * 512 + k: t5 * 512 + k + 512], k == 0, k == 30,
                                 r=["s3_dg"] + ukeys, w=[f"s3_pc{c}"])
                        B.act(Cf[:, c, :], pc[c][:, :], AF.Identity, r=[f"s3_pc{c}", "s3_vec"], w=["s3_Cf"], bias=vec[:, 0, c:c + 1])
                    B.tt("pool", Cs[:, :, :], Cf[:, :, :], Cf[:, :, :], OP.mult, r=["s3_Cf"], w=["s3_Cs"])
                    for c in range(3):
                        B.mm(pmean[:, :], ones[:, :], Cf[:, c, :], c == 0, c == 2, r=["s3_ones", "s3_Cf"], w=["s3_pmean"])
                    for c in range(3):
                        B.mm(pmsq[:, :], ones[:, :], Cs[:, c, :], c == 0, c == 2, r=["s3_ones", "s3_Cs"], w=["s3_pmsq"])
                    B.cp("act", mS[:, :], pmean[:, :], r=["s3_pmean"], w=["s3_mS"])
                    B.tt("pool", m2[:, :], mS[:, :], mS[:, :], OP.mult, r=["s3_mS"], w=["s3_m2"])
                    B.tt("dve", m2[:, :], pmsq[:, :], m2[:, :], OP.subtract, r=["s3_pmsq", "s3_m2"], w=["s3_m2"])
                    B.act(rs[:, :], m2[:, :], AF.Sqrt, r=["s3_m2"], w=["s3_rs"], bias=B.epsc[:, 1:2], scale=1.0)
                    B.P.op("dve", lambda e: e.reciprocal(rs[:, :], rs[:, :]), r=["s3_rs"], w=["s3_rs"])
                    for c in range(3):
                        B.tt("dve", y[:, :], Cf[:, c, :], mS[:, :], OP.subtract, r=["s3_Cf", "s3_mS"], w=["s3_y"])
                        B.tt("pool", y[:, :], y[:, :], rs[:, :], OP.mult, r=["s3_y", "s3_rs"], w=["s3_y"])
                        B.ts("dve", z[:, :], y[:, :], vec[:, 1, c:c + 1], OP.mult, r=["s3_y", "s3_vec"], w=["s3_z"], s2=vec[:, 2, c:c + 1], op1=OP.add)
                        B.act(y[:, :], z[:, :], AF.Sigmoid, r=["s3_z"], w=["s3_y"])
                        B.tt("dve", o[:, c, :], z[:, :], y[:, :], OP.mult, r=["s3_z", "s3_y"], w=[okk])
                    B.st_dram(self.mixT[256:640, tok0:tok0 + 512].rearrange("(c p) t -> p c t", p=128), o[:, :, :], r=[okk], w=[f"mixB_{tok0}"],
                              stream="st_" + okk, eng="pool")
        P.barrier()

    def stage_rwkv_prep(self):
        B, P, I = self, self.P, self.I
        S, NB = self.S, self.NB
        with ExitStack() as st:
            def vecload(name, ap, shape):
                t = B.sb(st, name, shape, F32)
                B.ld(t[tuple(slice(None) for _ in shape)], ap, r=[], w=[name], nc_ok=True)
                return t
            mu = B.c_mu
            B.tt("dve", mu[:, 0, :], mu[:, 1, :], mu[:, 2, :], OP.add, r=["s4_mu"], w=["s4_mu"])
            B.ts("dve", mu[:, 0, :], mu[:, 0, :], -1.0, OP.mult, r=["s4_mu"], w=["s4_mu"], s2=1.0, op1=OP.add)
            w0, a0, kkv, ka, rk = B.c_w0, B.c_a0, B.c_kkv, B.c_ka, B.c_rk
            omka = B.sb(st, "s4_omka", [128, 3], F32)
            B.ts("dve", omka[:, :], ka[:, :], -1.0, OP.mult, r=["s4_ka"], w=["s4_omka"], s2=1.0, op1=OP.add)
            dup = vecload("s4_dup", I["decay_up"], [128, 384])
            iup = vecload("s4_iup", I["iclr_up"], [128, 384])
            gup = vecload("s4_gup", I["gate_up"], [128, 384])
            rmask = B.sb(st, "s4_rmask", [128, 512], F32)
            B.memset("dve", rmask[:, :], 1.0, w=["s4_rmask"])
            B.memset("dve", rmask[:, :].rearrange("p (n s) -> p n s", s=64)[:, :, 0:1], 0.0, w=["s4_rmask"])
            L = B.sb(st, "s4_L", [128, 12, 514], F32)
            pfs = [B.sb(st, f"s4_pf{i}", [128, 12, 512], F32) for i in range(2)]
            TW = [B.sb(st, f"s4_tw{i}", [128, 512], F32) for i in range(2)]
            SG = [B.sb(st, f"s4_sgd{i}", [128, 512], F32) for i in range(2)]
            names = ["Gc", "kq", "sq", "kk", "sw", "a", "kd0", "kd1", "bb", "pre", "cum", "ex", "Ec", "Ei", "Ex", "t1"]
            alias = {"sd": "sq", "lw": "sw", "tmp": "t1", "bon": "Gc", "rt": "Ec", "at": "Ex", "bt": "bb", "kt": "Ei"}
            Xs = [{n: B.sb(st, f"s4_{n}_{i}", [128, 512], F32) for n in names} for i in range(3)]
            cur = [0]

            class _XV:
                def __getitem__(self, n):
                    return Xs[cur[0]][alias.get(n, n)]
            X = _XV()
            PLt = B.sb(st, "s4_PLt", [128, 8], F32)
            TMs = [B.sb(st, f"s4_TMs{i}", [128, 4, 128], F32) for i in range(2)]
            ps = [B.ps(st, f"s4_ps{i}", [128, 512], F32) for i in range(6)]
            pst = [B.ps(st, f"s4_pst{i}", [128, 4, 128], F32) for i in range(2)]
            cnt = {"ps": 0, "pst": 0}

            def K(n):
                return f"s4_{alias.get(n, n)}_{cur[0]}"

            def nps():
                i = cnt["ps"] % 6; cnt["ps"] += 1
                return ps[i], f"s4_ps{i}"

            def transpose_store(src, dst_ap, dkey):
                i = cnt["pst"] % 2; cnt["pst"] += 1
                for sub in range(4):
                    B.tr(pst[i][:, sub, :], X[src][:, sub * 128:(sub + 1) * 128], B.idf[:, :], r=[K(src), "c_idf"], w=[f"s4_pst{i}"])
                B.cp("act" if i else "dve", TMs[i][:, :, :], pst[i][:, :, :], r=[f"s4_pst{i}"], w=[f"s4_TMs{i}"])
                B.st_dram(dst_ap, TMs[i][:, :, :], r=[f"s4_TMs{i}"], w=[dkey])

            NEG = -0.606531
            active = []
            ntile = [0]

            def step_all():
                for g in list(active):
                    cur[0] = g[1]
                    try:
                        next(g[0])
                    except StopIteration:
                        active.remove(g)

            def launch(gen):
                while len(active) >= 3:
                    step_all()
                used = {g[1] for g in active}
                si = [i for i in range(3) if i not in used][0]
                active.append([gen, si])

            for b in range(NB):
                for t5 in range(S // 512):
                    tok0 = b * S + t5 * 512
                    tp = ntile[0] % 2
                    ntile[0] += 1
                    pf = pfs[tp]
                    pfk = (lambda j, tp=tp: f"s4_pf{tp}_{j}")
                    tw, sgd, twk, sgk = TW[tp], SG[tp], f"s4_tw{tp}", f"s4_sgd{tp}"
                    def loadL(b_, t5_):
                        tk0 = b_ * S + t5_ * 512
                        lo = 1 if t5_ == 0 else 0
                        hi = 513 if t5_ == S // 512 - 1 else 514
                        if lo:
                            B.memset("pool", L[:, :, 0:1], 0.0, w=["s4_L"])
                        if hi == 513:
                            B.memset("pool", L[:, :, 513:514], 0.0, w=["s4_L"])
                        for half in range(2):
                            B.ld(L[:, half * 6:(half + 1) * 6, lo:hi],
                                 self.pT[1024 + half * 768:1024 + (half + 1) * 768, tk0 - 1 + lo: tk0 - 1 + hi].rearrange("(c p) t -> p c t", p=128),
                                 r=[], w=["s4_L"], stream="s4_L")
                    if b == 0 and t5 == 0:
                        loadL(0, 0)
                    for c in range(12):
                        B.act(pf[:, c, :], L[:, c, 1:513], AF.Copy, r=["s4_L", "s4_mu"], w=[pfk(c)], scale=mu[:, 0, c:c + 1])
                        B.stt(pf[:, c, :], L[:, c, 0:512], mu[:, 1, c:c + 1], pf[:, c, :], OP.mult, OP.add, r=["s4_L", "s4_mu", pfk(c)], w=[pfk(c)])
                        B.stt(pf[:, c, :], L[:, c, 2:514], mu[:, 2, c:c + 1], pf[:, c, :], OP.mult, OP.add, r=["s4_L", "s4_mu", pfk(c)], w=[pfk(c)])
                    nxt = b * (S // 512) + t5 + 1
                    if nxt < NB * (S // 512):
                        loadL(nxt // (S // 512), nxt % (S // 512))
                    B.act(tw[:, :], pf[:, 9, :], AF.Tanh, r=[pfk(9)], w=[twk])
                    B.act(sgd[:, :], pf[:, 11, :], AF.Sigmoid, r=[pfk(11)], w=[sgk])
                    def citer(c, tok0=tok0, pf=pf, pfk=pfk, tw=tw, sgd=sgd, twk=twk, sgk=sgk):
                        cs = slice(c * 128, (c + 1) * 128)
                        rT, kT, vT = pf[:, c, :], pf[:, 3 + c, :], pf[:, 6 + c, :]
                        rK, kK, vK = pfk(c), pfk(3 + c), pfk(6 + c)
                        p, pk = nps()
                        B.mm(p[:, :], gup[:, cs], sgd[:, :], True, True, r=["s4_gup", sgk], w=[pk])
                        B.cp("act", X["Gc"][:, :], p[:, :], r=[pk], w=[K("Gc")])
                        B.st_dram(self.gT[cs, tok0:tok0 + 512], X["Gc"][:, :], r=[K("Gc")], w=[f"gT{tok0}_{c}"])
                        B.act(X["kq"][:, :], kT, AF.Copy, r=[kK, "s4_kkv"], w=[K("kq")], scale=kkv[:, c:c + 1])
                        B.act(X["sq"][:, :], kT, AF.Square, r=[kK, "s4_kkv"], w=[K("sq")], scale=kkv[:, c:c + 1])
                        yield
                        p, pk = nps()
                        B.mm(p[:, :], B.blk[:, :], X["sq"][:, :], True, True, r=["c_blk", K("sq")], w=[pk])
                        B.act(X["sd"][:, :], p[:, :], AF.Sqrt, r=[pk], w=[K("sd")], bias=B.epsc[:, 3:4], scale=1.0)
                        yield
                        B.P.op("dve", lambda e, t=X["sd"]: e.reciprocal(t[:, :], t[:, :]), r=[K("sd")], w=[K("sd")])
                        B.tt("dve", X["kk"][:, :], X["kq"][:, :], X["sd"][:, :], OP.mult, r=[K("kq"), K("sd")], w=[K("kk")])
                        yield
                        for d in range(2):
                            ds = slice(d * 64, (d + 1) * 64)
                            kd = f"kd{d}"
                            p, pk = nps()
                            B.mm(p[:, :], dup[ds, cs], tw[ds, :], True, True, r=["s4_dup", twk], w=[pk])
                            B.act(X["sw"][:, :], p[:, :], AF.Sigmoid, r=[pk, "s4_w0"], w=[K("sw")], bias=w0[:, d, c:c + 1], scale=1.0)
                            B.act(X["lw"][:, :], X["sw"][:, :], AF.Copy, r=[K("sw")], w=[K("lw")], scale=NEG)
                            yield
                            p, pk = nps()
                            B.mm(p[:, :], iup[ds, cs], pf[ds, 10, :], True, True, r=["s4_iup", pfk(10)], w=[pk])
                            B.act(X["a"][:, :], p[:, :], AF.Sigmoid, r=[pk, "s4_a0"], w=[K("a")], bias=a0[:, d, c:c + 1], scale=1.0)
                            yield
                            B.ts("dve", X["tmp"][:, :], X["a"][:, :], ka[:, c:c + 1], OP.mult, r=[K("a"), "s4_ka", "s4_omka"], w=[K("tmp")],
                                 s2=omka[:, c:c + 1], op1=OP.add)
                            B.tt("dve", X[kd][:, :], kT, X["tmp"][:, :], OP.mult, r=[kK, K("tmp")], w=[K(kd)])
                            B.tt("dve", X["bb"][:, :], X["kk"][:, :], X["a"][:, :], OP.mult, r=[K("kk"), K("a")], w=[K("bb")])
                            yield
                            B.P.op("dve", lambda e, t0=X["pre"], t1=X["lw"]: e.tensor_tensor_scan(t0[:, :], rmask[:, :], t1[:, :], 0.0, OP.mult, OP.add),
                                   r=["s4_rmask", K("lw")], w=[K("pre")])
                            yield
                            if d == 0:
                                cumk = "pre"
                                B.tt("dve", X["ex"][:, :], X["pre"][:, :], X["lw"][:, :], OP.subtract, r=[K("pre"), K("lw")], w=[K("ex")])
                            else:
                                cumk = "cum"
                                tot = X["pre"][:, :].rearrange("p (n s) -> p n s", s=64)[:, :, 63:64].to_broadcast([128, 8, 64])
                                B.tt("dve", X["ex"][:, :].rearrange("p (n s) -> p n s", s=64), tot,
                                     X["pre"][:, :].rearrange("p (n s) -> p n s", s=64), OP.subtract, r=[K("pre")], w=[K("ex")])
                                B.tt("dve", X["cum"][:, :], X["ex"][:, :], X["lw"][:, :], OP.add, r=[K("ex"), K("lw")], w=[K("cum")])
                            B.act(X["Ec"][:, :], X[cumk][:, :], AF.Exp, r=[K(cumk)], w=[K("Ec")])
                            B.act(X["Ei"][:, :], X[cumk][:, :], AF.Exp, r=[K(cumk)], w=[K("Ei")], scale=-1.0)
                            B.act(X["Ex"][:, :], X["ex"][:, :], AF.Exp, r=[K("ex")], w=[K("Ex")])
                            yield
                            B.stt(X["at"][:, :], X["kk"][:, :], -1.0, X["Ex"][:, :], OP.mult, OP.mult, r=[K("kk"), K("Ex")], w=[K("at")])
                            B.tt("dve", X["bt"][:, :], X["bb"][:, :], X["Ei"][:, :], OP.mult, r=[K("bb"), K("Ei")], w=[K("bt")])
                            B.tt("dve", X["kt"][:, :], X[kd][:, :], X["Ei"][:, :], OP.mult, r=[K(kd), K("Ei")], w=[K("kt")])
                            yield
                            pos = 63 if d == 0 else 0
                            B.cp("act", PLt[:, :], X["Ec"][:, :].rearrange("p (n s) -> p n s", s=64)[:, :, pos], r=[K("Ec")], w=["s4_PLt"])
                            B.tt("pool", X["rt"][:, :], rT, X["Ec"][:, :], OP.mult, r=[rK, K("Ec")], w=[K("rt")])
                            B.st_dram(self.PLd[d][cs, tok0 // 64: tok0 // 64 + 8], PLt[:, :], r=["s4_PLt"], w=[f"PL{d}_{tok0}_{c}"])
                            for qi, q in enumerate(["rt", "at", "bt", "kt"]):
                                B.st_dram(self.FM[d][qi, cs, tok0:tok0 + 512], X[q][:, :], r=[K(q)], w=[f"FM{d}_{qi}_{tok0}_{c}"])
                            for qi, q in enumerate(["at", "bt", "kt"]):
                                transpose_store(q, self.TM[d][tok0:tok0 + 512, qi, cs].rearrange("(s p) ch -> p s ch", p=128), f"TM{d}_{qi}_{tok0}_{c}")
                                yield
                        B.tt("pool", X["t1"][:, :], X["kd0"][:, :], X["kd1"][:, :], OP.add, r=[K("kd0"), K("kd1")], w=[K("t1")])
                        B.tt("pool", X["t1"][:, :], X["t1"][:, :], rT, OP.mult, r=[K("t1"), rK], w=[K("t1")])
                        B.act(X["t1"][:, :], X["t1"][:, :], AF.Copy, r=[K("t1"), "s4_rk"], w=[K("t1")], scale=rk[:, c:c + 1])
                        yield
                        p, pk = nps()
                        B.mm(p[:, :], B.blk[:, :], X["t1"][:, :], True, True, r=["c_blk", K("t1")], w=[pk])
                        B.tt("dve", X["bon"][:, :], p[:, :], vT, OP.mult, r=[pk, vK], w=[K("bon")])
                        B.st_dram(self.bonT[cs, tok0:tok0 + 512], X["bon"][:, :], r=[K("bon")], w=[f"bonT{tok0}_{c}"])
                        yield
                        i = cnt["pst"] % 2; cnt["pst"] += 1
                        for sub in range(4):
                            B.tr(pst[i][:, sub, :], pf[:, 6 + c, sub * 128:(sub + 1) * 128], B.idf[:, :], r=[vK, "c_idf"], w=[f"s4_pst{i}"])
                        B.cp("act" if i else "dve", TMs[i][:, :, :], pst[i][:, :, :], r=[f"s4_pst{i}"], w=[f"s4_TMs{i}"])
                        B.st_dram(self.TMV[tok0:tok0 + 512, cs].rearrange("(s p) ch -> p s ch", p=128), TMs[i][:, :, :], r=[f"s4_TMs{i}"], w=[f"TMV_{tok0}_{c}"])
                    for c in range(3):
                        launch(citer(c))
            while active:
                step_all()
        P.barrier()

    def stage_rwkv_scan(self):
        B, P, I = self, self.P, self.I
        S, NB = self.S, self.NB
        NP = S // 128
        with ExitStack() as st:
            MK = B.sb(st, "s5_mk", [128, 4, 128], F32)
            for i, op in enumerate([OP.is_ge, OP.is_gt, OP.is_le, OP.is_lt]):
                B.ts("dve", MK[:, i, :], B.dif[:, :], 0.0, op, r=["c_dif"], w=["s5_mk"])
                B.tt("dve", MK[:, i, :], MK[:, i, :], B.blk[:, :], OP.mult, r=["s5_mk", "c_blk"], w=["s5_mk"])
            FMt = [B.sb(st, f"s5_FM{i}", [64, 6, 4, 128], F32) for i in range(2)]
            TMt = [B.sb(st, f"s5_TM{i}", [128, 4 * 384 + 64], F32) for i in range(2)]
            H = [[B.sb(st, f"s5_H{d}_{h}", [64, 128], F32) for h in range(6)] for d in range(2)]
            BT = [B.sb(st, f"s5_BT_{h}", [128, 512], F32) for h in range(6)]
            E2 = [B.sb(st, f"s5_E2_{h}", [128, 256], F32) for h in range(6)]
            RhT = [B.sb(st, f"s5_Rh_{h}", [64, 128], F32) for h in range(6)]
            MT = [B.sb(st, f"s5_MT_{h}", [64, 2, 128], F32) for h in range(6)]
            Gp = [B.sb(st, f"s5_Gp_{h}", [64, 2, 64], F32) for h in range(6)]
            Y0 = [B.sb(st, f"s5_Y0_{h}", [128, 128], F32) for h in range(6)]
            YT = [B.sb(st, f"s5_YT_{h}", [128, 128], F32) for h in range(6)]
            PS = [B.ps(st, f"s5_ps{h}", [128, 512], F32) for h in range(6)]
            R = mybir.dt.float32r
            FMr = [B.sb(st, f"s5_FMr{i}", [64, 6, 4, 128], F32) for i in range(2)]
            TMr = [B.sb(st, f"s5_TMr{i}", [128, 3 * 384 + 64], F32) for i in range(2)]
            VZ = [B.sb(st, f"s5_VZ{i}", [128, 6, 128], F32) for i in range(2)]
            I2 = B.sb(st, "s5_i2", [64, 2, 64], F32)
            for c in range(2):
                B.cp("dve", I2[:, c, :], B.idf[0:64, 0:64], r=["c_idf"], w=["s5_i2"])
            ZR = B.sb(st, "s5_zr", [128, 768], F32)
            B.memset("dve", ZR[:, :], 0.0, w=["s5_zr"])
            Bbd = [B.sb(st, f"s5_Bbd{i}", [128, 6, 128], F32) for i in range(2)]
            Vbd = [B.sb(st, f"s5_Vbd{i}", [128, 6, 128], F32) for i in range(2)]
            W0bd = [B.sb(st, f"s5_W0bd{h}", [128, 128], F32) for h in range(6)]
            for i in range(2):
                B.cp("dve", Bbd[i][:, :, :].bitcast(R), ZR[:, :].rearrange("p (h i) -> p h i", i=128), r=["s5_zr"], w=[f"s5_Bbd{i}"])
                B.cp("dve", Vbd[i][:, :, :].bitcast(R), ZR[:, :].rearrange("p (h i) -> p h i", i=128), r=["s5_zr"], w=[f"s5_Vbd{i}"])
            for h in range(6):
                B.cp("dve", W0bd[h][:, :].bitcast(R), ZR[:, 0:128], r=["s5_zr"], w=[f"W0bd{h}"])
            for i in range(2):
                B.cp("dve", VZ[i][:, :, :].bitcast(R), ZR[:, :].rearrange("p (h i) -> p h i", i=128), r=["s5_zr"], w=[f"s5_VZ{i}"])
                B.cp("dve", TMr[i][:, 3 * 384:].bitcast(R), ZR[:, 0:64], r=["s5_zr"], w=[f"s5_TMr{i}"])
            for i in range(2):
                B.memset("pool", TMt[i][:, 4 * 384:], 0.0, w=[f"s5_TM{i}"])

            import os
            STOP = int(os.environ.get('CHAIN_STOP', '0'))

            def chain(b, d, p, h, gi):
                fm, tmf, fmo = FMr[gi], TMr[gi], FMt[gi]
                vz = VZ[gi]

                def tmq(rows, q, width=64):
                    if q == 3:
                        return vz[rows, h, 64:128].bitcast(R)
                    return tmf[rows, q * 384 + h * 64: q * 384 + h * 64 + width].bitcast(R)
                al = slice(0, 128)
                fk, tk, vk = f"s5_FMr{gi}", f"s5_TMr{gi}", f"s5_VZ{gi}"
                ps, pk = PS[h], f"s5_ps{h}"
                bt, e2 = BT[h], E2[h]
                kQT, kArb, kE2, kQ, kZ = f"QT{h}", f"Arb{h}", f"E2{h}", f"Q{h}", f"Z{h}"
                hs = slice(h * 64, (h + 1) * 64)
                mT = MK[:, 0:2, :] if d == 0 else MK[:, 2:4, :]
                mN = MK[:, 3, :] if d == 0 else MK[:, 1, :]
                tok0 = b * S + p * 128
                B.mm(ps[:, 0:256], fm[:, h, 2, :].bitcast(R), fm[:, h, 0:2, :].bitcast(R), True, True, r=[fk], w=[pk])
                B.mm(ps[:, 256:512], fm[:, h, 3, :].bitcast(R), fm[:, h, 0:2, :].bitcast(R), True, True, r=[fk], w=[pk])
                yield
                if STOP == 1: return
                B.tt("dve", bt[:, 0:256].bitcast(R).rearrange("p (a s) -> p a s", a=2), ps[:, 0:256].rearrange("p (a s) -> p a s", a=2), mT, OP.mult,
                     r=[pk, "s5_mk"], w=[kQT, kArb])
                B.tt("dve", e2[:, :].bitcast(R).rearrange("p (a s) -> p a s", a=2), ps[:, 256:512].rearrange("p (a s) -> p a s", a=2), mT, OP.mult,
                     r=[pk, "s5_mk"], w=[kE2])
                yield
                if STOP == 2: return
                B.mm(ps[:, 0:128], fm[:, h, 1, :].bitcast(R), fm[:, h, 2, :].bitcast(R), True, True, r=[fk], w=[pk])
                B.mm(ps[:, 128:192], e2[:, 128:256].bitcast(R), tmq(al, 3), True, True, r=[kE2, vk], w=[pk])
                yield
                if STOP == 3: return
                B.tt("dve", bt[:, 256:384].bitcast(R), ps[:, 0:128], mN, OP.mult, r=[pk, "s5_mk"], w=[pk, kQ])
                B.cp("act", bt[:, 448:512].bitcast(R), ps[:, 128:192], r=[pk], w=[pk, kZ])
                B.cp("act", bt[:, 384:448].bitcast(R), tmf[:, h * 64:(h + 1) * 64], r=[tk], w=[kZ])
                yield
                if STOP == 4: return
                for k in range(6):
                    if k < 5:
                        B.mm(ps[:, 128:384], bt[:, 128:256].bitcast(R), bt[:, 256:512].bitcast(R), True, True, r=[kQT, kQ, kZ], w=[pk])
                        B.mm(ps[:, 0:128], bt[:, 256:384].bitcast(R), bt[:, 128:256].bitcast(R), True, True, r=[kQT, kQ], w=[pk])
                    else:
                        B.mm(ps[:, 256:384], bt[:, 128:256].bitcast(R), bt[:, 384:512].bitcast(R), True, True, r=[kQT, kZ], w=[pk])
                    yield
                    if STOP == 5: return
                    B.tt("dve", bt[:, 384:512].bitcast(R), ps[:, 256:384], bt[:, 384:512], OP.add, r=[pk, kZ], w=[pk, kZ])
                    if k < 5:
                        B.cp("act", bt[:, 128:384].bitcast(R), ps[:, 0:256], r=[pk], w=[pk, kQ, kQT])
                    yield
                    if STOP == 6: return
                for c in range(2):
                    cr = slice(c * 64, (c + 1) * 64)
                    B.cp("act", W0bd[h][cr, c * 64:(c + 1) * 64].bitcast(R), bt[cr, 448:512], r=[kZ], w=[f"W0bd{h}"])
                B.mm(ps[:, 0:128], bt[:, 384:512].bitcast(R), bt[:, 0:128].bitcast(R), True, False, r=[kZ, kArb], w=[pk])
                B.mm(ps[:, 0:128], vz[:, h, :].bitcast(R), e2[:, 0:128].bitcast(R), False, True, r=[vk, kE2], w=[pk])
                B.mm(ps[:, 128:256], bt[:, 384:512].bitcast(R), Bbd[gi][:, h, :].bitcast(R), True, True, r=[kZ, f"s5_Bbd{gi}"], w=[pk])
                B.mm(ps[:, 256:384], tmq(al, 1, 128), W0bd[h][:, :].bitcast(R), True, False, r=[tk, f"W0bd{h}"], w=[pk])
                B.mm(ps[:, 256:384], tmq(al, 2, 128), Vbd[gi][:, h, :].bitcast(R), False, True, r=[tk, f"s5_Vbd{gi}"], w=[pk])
                yield
                if STOP == 7: return
                B.tt("dve", RhT[h][:, :].bitcast(R), ps[0:64, 0:128], fmo[:, h, 0, :], OP.add, r=[pk, f"s5_FM{gi}"], w=[pk, f"Rh{h}"])
                B.tt("dve", MT[h][:, :, 0:64].bitcast(R), ps[0:64, 128:256].rearrange("p (c j) -> p c j", c=2), I2[:, :, :], OP.add, r=[pk, "s5_i2"], w=[pk, f"MT{h}"])
                for c in range(2):
                    n = p * 2 + c
                    B.act(Gp[h][:, c, :], ps[0:64, 256 + c * 64:320 + c * 64], AF.Copy, r=[pk, f"s5_PL{b}_{d}"], w=[pk, f"Gp{h}"], scale=PLtb[b][d][:, h, n:n + 1])
                B.cp("act", Y0[h][64:128, :], ps[64:128, 0:128], r=[pk], w=[pk, f"Y0{h}"])
                yield
                if STOP == 8: return
                Hs, hk = H[d][h], f"H{d}_{h}"
                for c in ((0, 1) if d == 0 else (1, 0)):
                    n = p * 2 + c
                    cc = slice(c * 64, (c + 1) * 64)
                    B.mm(ps[:, 0:64], Hs[:, :].bitcast(R), RhT[h][:, cc].bitcast(R), True, True, r=[hk, f"Rh{h}"], w=[pk])
                    B.mm(ps[:, 64:128], MT[h][:, c, :].bitcast(R), Hs[:, 64:128].bitcast(R), True, True, r=[hk, f"MT{h}"], w=[pk])
                    yield
                    if STOP == 9: return
                    B.tt("dve", YT[h][64:128, cc], ps[64:128, 0:64], Y0[h][64:128, cc], OP.add, r=[pk, f"Y0{h}"], w=[pk, f"YT{h}"])
                    B.stt(Hs[:, 64:128].bitcast(R), ps[0:64, 64:128], PLtb[b][d][:, h, n:n + 1], Gp[h][:, c, :], OP.mult, OP.add,
                          r=[pk, f"s5_PL{b}_{d}", f"Gp{h}"], w=[pk, hk])
                    yield
                    if STOP == 10: return
                B.st_dram(self.YT[d][hs, tok0:tok0 + 128], YT[h][64:128, :], r=[f"YT{h}"], w=[f"YTd{d}_{h}_{tok0}"], stream=f"st_YT{h}")

            groups = []
            for b in range(NB):
                for i in range(NP):
                    for d in range(2):
                        groups.append((b, d, i if d == 0 else NP - 1 - i))

            def prologue(g):
                b, d, p = groups[g]
                gi = g % 2
                tok0 = b * S + p * 128
                for q in range(4):
                    B.ld(FMt[gi][:, :, q, :], self.FM[d][q, :, tok0:tok0 + 128].rearrange("(h j) t -> j h t", j=64), r=[], w=[f"s5_FM{gi}"],
                         stream=f"s5_FM{gi}")
                B.ld(TMt[gi][:, 0:3 * 384], self.TM[d][tok0:tok0 + 128, :, :].rearrange("t q c -> t (q c)"), r=[], w=[f"s5_TM{gi}"], stream=f"s5_TM{gi}")
                B.ld(TMt[gi][:, 3 * 384:4 * 384], self.TMV[tok0:tok0 + 128, :], r=[], w=[f"s5_TM{gi}"], stream=f"s5_TM{gi}")
                B.cp("pool", FMr[gi][:, :, :, :].bitcast(R), FMt[gi][:, :, :, :], r=[f"s5_FM{gi}"], w=[f"s5_FMr{gi}"])
                B.cp("pool", TMr[gi][:, 0:3 * 384].bitcast(R), TMt[gi][:, 0:3 * 384], r=[f"s5_TM{gi}"], w=[f"s5_TMr{gi}"])
                B.cp("pool", VZ[gi][:, :, 64:128].bitcast(R), TMt[gi][:, 3 * 384:4 * 384].rearrange("p (h i) -> p h i", i=64), r=[f"s5_TM{gi}"], w=[f"s5_VZ{gi}"])
                for c in range(2):
                    cr = slice(c * 64, (c + 1) * 64)
                    B.cp("pool", Bbd[gi][cr, :, c * 64:(c + 1) * 64].bitcast(R), TMt[gi][cr, 384:768].rearrange("p (h i) -> p h i", i=64),
                         r=[f"s5_TM{gi}"], w=[f"s5_Bbd{gi}"])
                    B.cp("pool", Vbd[gi][cr, :, c * 64:(c + 1) * 64].bitcast(R), TMt[gi][cr, 3 * 384:4 * 384].rearrange("p (h i) -> p h i", i=64),
                         r=[f"s5_TM{gi}"], w=[f"s5_Vbd{gi}"])

            PLtb = [[B.sb(st, f"s5_PLb{b}_{d}", [64, 6, S // 64], F32) for d in range(2)] for b in range(NB)]
            for b in range(NB):
                for d in range(2):
                    B.ld(PLtb[b][d][:, :, :], self.PLd[d][:, b * (S // 64):(b + 1) * (S // 64)].rearrange("(h j) c -> j h c", j=64), r=[], w=[f"s5_PL{b}_{d}"])
            prologue(0)
            for g, (b, d, p) in enumerate(groups):
                if p == (0 if d == 0 else NP - 1):
                    for h in range(6):
                        B.cp("dve", H[d][h][:, :].bitcast(R), ZR[0:64, 0:128], r=["s5_zr"], w=[f"H{d}_{h}"])
                        if g == 0:
                            B.cp("dve", MT[h][:, :, :].bitcast(R), ZR[0:64, 0:256].rearrange("p (c j) -> p c j", c=2), r=["s5_zr"], w=[f"MT{h}"])
                if g + 1 < len(groups):
                    prologue(g + 1)
                gens = [chain(b, d, p, h, g % 2) for h in range(6)]
                while gens:
                    for gg in list(gens):
                        try:
                            next(gg)
                        except StopIteration:
                            gens.remove(gg)
        P.barrier()

    def stage_rwkv_post(self):
        B, P, I = self, self.P, self.I
        S, NB = self.S, self.NB
        with ExitStack() as st:
            vec = B.sb(st, "s6_vec", [128, 2, 3], F32)
            for j, nm in enumerate(["lnx_g", "lnx_b"]):
                B.ld(vec[:, j, :], I[nm].rearrange("(c p) -> p c", p=128), r=[], w=["s6_vec"], nc_ok=True)
            bm = B.sb(st, "s6_bm", [128, 128], F32)
            B.ts("dve", bm[:, :], B.blk[:, :], 1.0 / 64.0, OP.mult, r=["c_blk"], w=["s6_bm"])
            names = ["yf", "yb", "g", "bon", "y", "sq", "mS", "m2", "rs", "z"]
            NS6 = 3
            Xs = [{n: B.sb(st, f"s6_{n}{i}", [128, 512], F32) for n in names} for i in range(NS6)]
            ob = [B.sb(st, f"s6_ob{i}", [128, 512], BF16) for i in range(NS6)]
            p1 = [B.ps(st, f"s6_p1{i}", [128, 512], F32) for i in range(NS6)]
            p2 = [B.ps(st, f"s6_p2{i}", [128, 512], F32) for i in range(NS6)]
            its = [(tok0, c) for tok0 in range(0, NB * S, 512) for c in range(3)]

            def comp(i, j):
                tok0, c = its[i]
                X = Xs[j]
                cs = slice(c * 128, (c + 1) * 128)
                ts_ = slice(tok0, tok0 + 512)
                K = lambda n: f"s6_{n}{j}"
                for n, src in (("yf", self.YT[0]), ("yb", self.YT[1]), ("g", self.gT), ("bon", self.bonT)):
                    B.ld(X[n][:, :], src[cs, ts_], r=[], w=[K(n)])
                yield
                P1, P2, k1, k2 = p1[j], p2[j], f"s6_p1{j}", f"s6_p2{j}"
                B.tt("pool", X["y"][:, :], X["yf"][:, :], X["yb"][:, :], OP.add, r=[K("yf"), K("yb")], w=[K("y")])
                yield
                B.tt("dve", X["sq"][:, :], X["y"][:, :], X["y"][:, :], OP.mult, r=[K("y")], w=[K("sq")])
                B.mm(P1[:, :], bm[:, :], X["y"][:, :], True, True, r=["s6_bm", K("y")], w=[k1])
                yield
                B.mm(P2[:, :], bm[:, :], X["sq"][:, :], True, True, r=["s6_bm", K("sq")], w=[k2])
                B.cp("act", X["mS"][:, :], P1[:, :], r=[k1], w=[K("mS")])
                yield
                B.act(X["m2"][:, :], X["mS"][:, :], AF.Square, r=[K("mS")], w=[K("m2")])
                B.tt("dve", X["y"][:, :], X["y"][:, :], X["mS"][:, :], OP.subtract, r=[K("y"), K("mS")], w=[K("y")])
                yield
                B.tt("dve", X["m2"][:, :], P2[:, :], X["m2"][:, :], OP.subtract, r=[k2, K("m2")], w=[K("m2")])
                yield
                B.act(X["rs"][:, :], X["m2"][:, :], AF.Sqrt, r=[K("m2")], w=[K("rs")], bias=B.epsc[:, 2:3], scale=1.0)
                yield
                B.P.op("dve", lambda e, t=X["rs"]: e.reciprocal(t[:, :], t[:, :]), r=[K("rs")], w=[K("rs")])
                B.tt("dve", X["y"][:, :], X["y"][:, :], X["rs"][:, :], OP.mult, r=[K("y"), K("rs")], w=[K("y")])
                B.ts("dve", X["z"][:, :], X["y"][:, :], vec[:, 0, c:c + 1], OP.mult, r=[K("y"), "s6_vec"], w=[K("z")], s2=vec[:, 1, c:c + 1], op1=OP.add)
                yield
                B.tt("pool", X["z"][:, :], X["z"][:, :], X["bon"][:, :], OP.add, r=[K("z"), K("bon")], w=[K("z")])
                yield
                o, okk = ob[j], f"s6_ob{j}"
                B.tt("dve", o[:, :], X["z"][:, :], X["g"][:, :], OP.mult, r=[K("z"), K("g")], w=[okk])
                B.st_dram(self.mixT[640 + c * 128: 640 + (c + 1) * 128, ts_], o[:, :], r=[okk], w=[f"mixC{tok0}_{c}"], eng="pool")

            active = []

            def step_all():
                for g in list(active):
                    try:
                        next(g[0])
                    except StopIteration:
                        active.remove(g)

            for i in range(len(its)):
                while len(active) >= NS6:
                    step_all()
                used = {g[1] for g in active}
                j = [x for x in range(NS6) if x not in used][0]
                active.append([comp(i, j), j])
                step_all()
            while active:
                step_all()
        P.barrier()

    def stage_outproj_router(self, st2):
        B, P, I = self, self.P, self.I
        S, NB = self.S, self.NB
        self.NPK = 16 if NB == 1 else 48
        self.affT = B.sb(st2, "affT", [self.NPK, S], F32)
        B.memset("pool", self.affT[:, :], 0.0, w=["affT"])
        with ExitStack() as st:
            W = B.sb(st, "s7_w", [128, 8, D], BF16)
            B.ld(W[:, :, :], I["w_out"].rearrange("(kc p) n -> p kc n", p=128), r=[], w=["s7_w"], eng="pool")
            Wr = B.sb(st, "s7_wr", [128, 8, E], F32)
            B.ld(Wr[:, :, :], I["w_router"].rearrange("(kc p) n -> p kc n", p=128), r=[], w=["s7_wr"], nc_ok=True)
            gB = B.sb(st, "s7_g", [128, D], F32)
            B.ld(gB[:, :], I["norm_ffn_g"].partition_broadcast(128), r=[], w=["s7_g"])
            mx = [B.sb(st, f"s7_mx{i}", [128, 8, 512], BF16) for i in range(2)]
            xt = [B.sb(st, f"s7_x{i}", [128, D], F32) for i in range(2)]
            x1 = [B.sb(st, f"s7_x1{i}", [128, D], F32) for i in range(2)]
            h2 = B.sb(st, "s7_h2", [128, D], F32)
            h2b = [B.sb(st, f"s7_h2b{i}", [128, D], BF16) for i in range(2)]
            h2T = B.sb(st, "s7_h2T", [128, 8, 128], F32)
            junk = B.sb(st, "s7_junk", [128, D], BF16)
            ss = B.sb(st, "s7_ss", [128, 4], F32)
            sm = B.sb(st, "s7_sm", [128, 4], F32)
            ex = B.sb(st, "s7_ex", [128, E], F32)
            aff = B.sb(st, "s7_aff", [128, 48], F32)
            B.memset("pool", aff[:, :], 0.0, w=["s7_aff"])
            po = [B.ps(st, f"s7_po{i}", [128, 512], F32) for i in range(2)]
            pt = [B.ps(st, f"s7_pt{i}", [128, 4, 128], F32) for i in range(2)]
            pl = B.ps(st, "s7_pl", [128, 512], F32)
            pa = B.ps(st, "s7_pa", [128, 512], F32)
            h2s = [h2, B.sb(st, "s7_h2_1", [128, D], F32)]
            h2k = ["s7_h2", "s7_h2_1"]
            sss = [ss, B.sb(st, "s7_ss1", [128, 4], F32)]
            ssk = ["s7_ss", "s7_ss1"]
            NT7 = NB * S // 128

            def ld7(it_):
                j2, tk = it_ % 2, it_ * 128
                if it_ % 4 == 0:
                    g2 = (it_ // 4) % 2
                    B.ld(mx[g2][:, :, :], self.mixT[:, tk:tk + 512].rearrange("(c p) t -> p c t", p=128), r=[], w=[f"s7_mx{g2}"])
                B.ld(xt[j2][:, :], I["x"][tk:tk + 128, :], r=[], w=[f"s7_x{j2}"])

            def phaseA(it):
                tok0 = it * 128
                i2 = it % 2
                if it == 0:
                    ld7(0)
                if it + 1 < NT7:
                    ld7(it + 1)
                for half in range(2):
                    for kc in range(8):
                        g2, o4 = (it // 4) % 2, (it % 4) * 128
                        B.mm(po[half][:, :], mx[g2][:, kc, o4:o4 + 128], W[:, kc, half * 512:(half + 1) * 512], kc == 0, kc == 7,
                             r=[f"s7_mx{g2}", "s7_w"], w=[f"s7_po{half}"])
                    B.tt("dve", x1[i2][:, half * 512:(half + 1) * 512], po[half][:, :], xt[i2][:, half * 512:(half + 1) * 512], OP.add,
                         r=[f"s7_po{half}", f"s7_x{i2}"], w=[f"s7_x1{i2}"])
                B.st_dram(self.x1d[tok0:tok0 + 128, :], x1[i2][:, :], r=[f"s7_x1{i2}"], w=[f"x1d{tok0}"])

            def phaseA2(it):
                tok0 = it * 128
                i2 = it % 2
                B.rmsnorm_rows(x1[i2][:, :], f"s7_x1{i2}", gB[:, :], "s7_g", h2s[i2][:, :], h2k[i2], sss[i2], ssk[i2], junk)
                B.cp("act", h2b[i2][:, :], h2s[i2][:, :], r=[h2k[i2]], w=[f"s7_h2b{i2}"])
                B.st_dram(self.h2d[tok0:tok0 + 128, :], h2b[i2][:, :], r=[f"s7_h2b{i2}"], w=[f"h2d{tok0}"])

            def phaseB(it):
                tok0 = it * 128
                b, t_in = tok0 // S, tok0 % S
                i2 = it % 2
                for q in range(2):
                    for k4 in range(4):
                        kc = q * 4 + k4
                        B.tr(pt[q][:, k4, :], h2s[i2][:, kc * 128:(kc + 1) * 128], B.idf[:, :], r=[h2k[i2], "c_idf"], w=[f"s7_pt{q}"])
                    B.cp("act" if q else "dve", h2T[:, q * 4:(q + 1) * 4, :], pt[q][:, :, :], r=[f"s7_pt{q}"], w=["s7_h2T"])
                for kc in range(8):
                    B.mm(pl[:, 0:E], h2T[:, kc, :], Wr[:, kc, :], kc == 0, kc == 7, r=["s7_h2T", "s7_wr"], w=["s7_pl"])

            def phaseB2(it):
                tok0 = it * 128
                b, t_in = tok0 // S, tok0 % S
                B.P.op("dve", lambda e: e.reduce_max(sm[:, 0:1], pl[:, 0:E], axis=mybir.AxisListType.X), r=["s7_pl"], w=["s7_sm0"])
                B.ts("dve", sm[:, 1:2], sm[:, 0:1], -1.0, OP.mult, r=["s7_sm0"], w=["s7_sm1"])
                B.act(ex[:, :], pl[:, 0:E], AF.Exp, r=["s7_pl", "s7_sm1"], w=["s7_ex", "s7_sm2"], bias=sm[:, 1:2], scale=1.0, accum_out=sm[:, 2:3])
                B.P.op("dve", lambda e: e.reciprocal(sm[:, 3:4], sm[:, 2:3]), r=["s7_sm2"], w=["s7_sm3"])
                B.ts("dve", aff[:, b * 32:b * 32 + E], ex[:, :], sm[:, 3:4], OP.mult, r=["s7_ex", "s7_sm3"], w=["s7_aff"])
                B.tr(pa[0:48, 0:128], aff[:, :], B.idf[:, :], r=["s7_aff", "c_idf"], w=["s7_pa"])
                B.cp("act", self.affT[b * 32:b * 32 + E, t_in:t_in + 128], pa[b * 32:b * 32 + E, 0:128], r=["s7_pa"], w=["affT"])

            for it in range(NT7 + 2):
                if it < NT7:
                    phaseA(it)
                if 0 <= it - 2 < NT7:
                    phaseB2(it - 2)
                if 0 <= it - 1 < NT7:
                    phaseB(it - 1)
                if it < NT7:
                    phaseA2(it)
        P.barrier()

    def stage_topk(self, st2):
        B, P = self, self.P
        S, NB = self.S, self.NB
        cap = 2 * S // E
        SC = min(128, cap)
        nsc = cap // SC
        NPK = self.NPK
        self.cap, self.SC, self.nsc = cap, SC, nsc
        self.idxT = B.sb(st2, "idxT", [128, NB, nsc, E], mybir.dt.int32)
        self.gatT = B.sb(st2, "gatT", [128, NB, nsc, E], F32)
        with ExitStack() as st:
            work = B.sb(st, "s8_work", [NPK, S], F32)
            vals = B.sb(st, "s8_vals", [NPK, cap], F32)
            idxu = B.sb(st, "s8_idxu", [NPK, cap], U32)
            idxf = B.sb(st, "s8_idxf", [NPK, cap], F32)
            pp = B.ps(st, "s8_pp", [128, 512], F32)
            B.cp("dve", work[:, :], self.affT[:, :], r=["affT"], w=["s8_work"])
            for r_ in range(cap // 8):
                sl = slice(r_ * 8, (r_ + 1) * 8)
                B.P.op("dve", lambda e, sl=sl: e.max(vals[:, sl], work[:, :]), r=["s8_work"], w=["s8_vals"])
                B.P.op("dve", lambda e, sl=sl: e.max_index(idxu[:, sl], vals[:, sl], work[:, :]), r=["s8_work", "s8_vals"], w=["s8_idxu"])
                B.P.op("dve", lambda e, sl=sl: e.match_replace(work[:, :], vals[:, sl], work[:, :], -1.0), r=["s8_work", "s8_vals"], w=["s8_work"])
            B.cp("dve", idxf[:, :], idxu[:, :], r=["s8_idxu"], w=["s8_idxf"])
            for b in range(1, NB):
                B.ts("dve", idxf[b * 32:b * 32 + E, :], idxf[b * 32:b * 32 + E, :], float(b * S), OP.add, r=["s8_idxf"], w=["s8_idxf"])
            for sc in range(nsc):
                B.tr(pp[0:SC, 0:NPK], idxf[:, sc * SC:(sc + 1) * SC], B.idf[0:NPK, 0:NPK], r=["s8_idxf", "c_idf"], w=["s8_pp"])
                for b in range(NB):
                    B.cp("dve", self.idxT[0:SC, b, sc, :], pp[0:SC, b * 32:b * 32 + E], r=["s8_pp"], w=["s8_pp", "idxT"])
                B.tr(pp[0:SC, 0:NPK], vals[:, sc * SC:(sc + 1) * SC], B.idf[0:NPK, 0:NPK], r=["s8_vals", "c_idf"], w=["s8_pp"])
                for b in range(NB):
                    B.cp("dve", self.gatT[0:SC, b, sc, :], pp[0:SC, b * 32:b * 32 + E], r=["s8_pp"], w=["s8_pp", "gatT"])
        P.barrier()

    def experts_prefetch(self, st2):
        B, I = self, self.I
        self.Wg = [B.sb(st2, f"s9_wg{i}", [128, 8, FF], BF16) for i in range(2)]
        self.Wu = [B.sb(st2, f"s9_wu{i}", [128, 8, FF], BF16) for i in range(2)]
        self.Wd = [B.sb(st2, f"s9_wd{i}", [128, 8, D], BF16) for i in range(2)]
        for nm, Wt, src in (("wg", self.Wg, "w_e_gate"), ("wu", self.Wu, "w_e_up"), ("wd", self.Wd, "w_e_down")):
            B.ld(Wt[0][:, :, :], I[src][0].rearrange("(kc p) n -> p kc n", p=128), r=[], w=[f"s9_{nm}0"], eng="pool", stream=f"s9pre_{nm}")

    def stage_experts(self):
        B, P, I = self, self.P, self.I
        S, NB = self.S, self.NB
        cap, SC, nsc = self.cap, self.SC, self.nsc
        with ExitStack() as st:
            Wg, Wu, Wd = self.Wg, self.Wu, self.Wd
            XEs = [B.sb(st, f"s9_xe{i}", [128, nsc, D], BF16) for i in range(2)]
            XTs = [B.sb(st, f"s9_xt{i}", [128, 8, cap], BF16) for i in range(2)]
            hTs = [B.sb(st, f"s9_hT{i}", [128, 8, cap], BF16) for i in range(2)]
            sg = B.sb(st, "s9_sg", [128, cap], F32)
            YE = [B.sb(st, f"s9_ye{i}", [128, D], F32) for i in range(4)]
            pst = [B.ps(st, f"s9_pt{i}", [128, 1024], BF16) for i in range(2)]
            pg = [B.ps(st, f"s9_pg{i}", [128, 512], F32) for i in range(2)]
            pu = [B.ps(st, f"s9_pu{i}", [128, 512], F32) for i in range(2)]
            pd = [B.ps(st, f"s9_pd{i}", [128, 512], F32) for i in range(2)]
            nye = 0
            def loadw(e):
                w2 = e % 2
                for nm, Wt, src in (("wg", Wg, "w_e_gate"), ("wu", Wu, "w_e_up"), ("wd", Wd, "w_e_down")):
                    B.ld(Wt[w2][:, :, :], I[src][e].rearrange("(kc p) n -> p kc n", p=128), r=[], w=[f"s9_{nm}{w2}"], eng="pool")
            def gather(it):
                e_, b_ = it // NB, it % NB
                j2 = it % 2
                for sc in range(nsc):
                    B.P.dma("pool", lambda eng, sc=sc, b_=b_, e_=e_, j2=j2: eng.indirect_dma_start(
                        out=XEs[j2][0:SC, sc, :], out_offset=None, in_=self.h2d[:, :],
                        in_offset=bass.IndirectOffsetOnAxis(ap=self.idxT[0:SC, b_, sc, e_:e_ + 1], axis=0)),
                        r=["idxT"], w=[f"s9_xe{j2}"], stream=f"s9_xe{j2}")
            for e in range(E):
                w2 = e % 2
                if e + 1 < E:
                    loadw(e + 1)
                for b in range(NB):
                    it = e * NB + b
                    i2 = it % 2
                    XE, XT, hT = XEs[i2], XTs[i2], hTs[i2]
                    kxe, kxt, khT = f"s9_xe{i2}", f"s9_xt{i2}", f"s9_hT{i2}"
                    if it == 0:
                        gather(0)
                    if it + 1 < E * NB:
                        gather(it + 1)
                    for kc in range(8):
                        pt_ = pst[kc % 2]; ptk = f"s9_pt{kc % 2}"
                        for sc in range(nsc):
                            B.tr(pt_[:, sc * SC:(sc + 1) * SC], XE[0:SC, sc, kc * 128:(kc + 1) * 128], B.idb[0:SC, 0:SC], r=[kxe, "c_idb"], w=[ptk])
                        B.cp("act" if kc % 2 else "dve", XT[:, kc, :], pt_[:, 0:cap], r=[ptk], w=[kxt])
                    for fc in range(8):
                        f2 = fc % 2
                        fs = slice(fc * 128, (fc + 1) * 128)
                        for kc in range(8):
                            B.mm(pg[f2][:, 0:cap], Wg[w2][:, kc, fs], XT[:, kc, :], kc == 0, kc == 7, r=[f"s9_wg{w2}", kxt], w=[f"s9_pg{f2}"])
                        for kc in range(8):
                            B.mm(pu[f2][:, 0:cap], Wu[w2][:, kc, fs], XT[:, kc, :], kc == 0, kc == 7, r=[f"s9_wu{w2}", kxt], w=[f"s9_pu{f2}"])
                        B.act(sg[:, :], pg[f2][:, 0:cap], AF.Silu, r=[f"s9_pg{f2}"], w=["s9_sg"])
                        B.tt("dve", hT[:, fc, :], pu[f2][:, 0:cap], sg[:, :], OP.mult, r=[f"s9_pu{f2}", "s9_sg"], w=[khT])
                    for sc in range(nsc):
                        ye = YE[nye % 4]; yk = f"s9_ye{nye % 4}"; nye += 1
                        for dh in range(2):
                            for fc in range(8):
                                B.mm(pd[dh][0:SC, :], hT[:, fc, sc * SC:(sc + 1) * SC], Wd[w2][:, fc, dh * 512:(dh + 1) * 512], fc == 0, fc == 7,
                                     r=[khT, f"s9_wd{w2}"], w=[f"s9_pd{dh}"])
                            B.ts("dve", ye[0:SC, dh * 512:(dh + 1) * 512], pd[dh][0:SC, :], self.gatT[0:SC, b, sc, e:e + 1], OP.mult,
                                 r=[f"s9_pd{dh}", "gatT"], w=[yk])
                        B.P.dma("pool", lambda eng, ye=ye, sc=sc, b=b, e=e: eng.indirect_dma_start(
                            out=self.x1d[:, :], out_offset=bass.IndirectOffsetOnAxis(ap=self.idxT[0:SC, b, sc, e:e + 1], axis=0),
                            in_=ye[0:SC, :], in_offset=None, compute_op=OP.add),
                            r=[yk, "idxT"], w=["x1d_acc"], stream="st_" + yk)
        P.barrier()

    def stage_final(self):
        B, P, I = self, self.P, self.I
        with ExitStack() as st:
            gB = B.sb(st, "s10_g", [128, D], F32)
            B.ld(gB[:, :], I["final_norm_g"].partition_broadcast(128), r=[], w=["s10_g"])
            xt = [B.sb(st, f"s10_x{i}", [128, D], F32) for i in range(2)]
            ot = [B.sb(st, f"s10_o{i}", [128, D], F32) for i in range(2)]
            tm10 = [B.sb(st, f"s10_t{i}", [128, D], F32) for i in range(2)]
            junk = B.sb(st, "s10_junk", [128, D], BF16)
            ss = [B.sb(st, f"s10_ss{i}", [128, 4], F32) for i in range(2)]
            for it in range(self.T // 128):
                i2 = it % 2
                if it == 0:
                    B.ld(xt[0][:, :], self.x1d[0:128, :], r=[], w=["s10_x0"])
                if it + 1 < self.T // 128:
                    B.ld(xt[1 - i2][:, :], self.x1d[(it + 1) * 128:(it + 2) * 128, :], r=[], w=[f"s10_x{1 - i2}"])
                B.rmsnorm_rows(xt[i2][:, :], f"s10_x{i2}", gB[:, :], "s10_g", ot[i2][:, :], f"s10_o{i2}", ss[i2], f"s10_ss{i2}", junk,
                               tmp=tm10[i2][:, :], tmpk=f"s10_t{i2}")
                B.st_dram(self.out[it * 128:(it + 1) * 128, :], ot[i2][:, :], r=[f"s10_o{i2}"], w=[f"out{it}"])
        P.barrier()


def build(S, NB, debug=False, upto=99):
    B = Builder(S, NB, debug)
    B.declare_io()
    with B.stack:
        cst = B.stack.enter_context(ExitStack())
        B.consts(cst)
        B.P.barrier()
        B.stage_inproj()
        if upto >= 2:
            B.stage_attn()
        if upto >= 3:
            B.stage_conv()
        if upto >= 4:
            B.stage_rwkv_prep()
        if upto >= 5:
            B.stage_rwkv_scan()
        if upto >= 6:
            B.stage_rwkv_post()
        if upto >= 7:
            st2 = B.stack.enter_context(ExitStack())
            B.stage_outproj_router(st2)
        if upto >= 9:
            B.experts_prefetch(st2)
        if upto >= 8:
            B.stage_topk(st2)
        if upto >= 9:
            B.stage_experts()
            B.stage_final()
        B.P.emit()
    return B


def prep(inp, b0, NB):
    m = {}
    for k, v in inp.items():
        v = np.asarray(v)
        if k == "x":
            m[k] = np.ascontiguousarray(v[b0:b0 + NB].reshape(NB * v.shape[1], D))
        elif k == "mem":
            m[k] = np.ascontiguousarray(v[b0:b0 + NB].reshape(NB * NMEM, D))
        elif k == "final_norm_g":
            m[k] = np.ascontiguousarray(v)
        else:
            a = v[0]
            if k in ("decay_up", "iclr_up"):
                a = a.reshape(128, 384)
            if k == "r_k":
                a = a.reshape(384)
            m[k] = np.ascontiguousarray(a)
    return m


_CACHE = {}


def kernel(**inputs):
    S, NB, NC = 4096, 2, 8
    if "B" not in _CACHE:
        _CACHE["B"] = build(S, NB, debug=False)
    B = _CACHE["B"]
    in_maps = [prep(inputs, c * NB, NB) for c in range(NC)]
    res = run_bass_kernel_spmd(B.nc, in_maps, core_ids=list(range(NC)))
    outs = [np.asarray(r["out"], dtype=np.float32).reshape(NB, S, D) for r in res.results]
    return np.concatenate(outs, axis=0)
```
